# Optimizing a Trainium2 kernel written in Bass

```python
import jax, jax.numpy as jnp
from jax import lax
import numpy as np

D_MODEL = 2048
BATCH = 2
SEQ = 4096
DEPTH = 1

N_HEADS_MOBA = 8
HEAD_DIM_MOBA = 128
MOBA_BLOCK = 256
MOBA_TOPK = 3
MOBA_Q_CHUNK = 32
N_HEADS_RET = 8
HEAD_DIM_RET_QK = 128
HEAD_DIM_RET_V = 256
RET_CHUNK = 128
ROPE_BASE = 10000.0
N_GROUPS = 4
EXPERTS_PER_GROUP = 8
N_EXPERTS = N_GROUPS * EXPERTS_PER_GROUP
EXPERT_TOPK = 2
D_EXPERT = 512

RMS_EPS = 1e-6
GN_EPS = 1e-6
NEG_BIG = -1e30

W_MOBA = N_HEADS_MOBA * HEAD_DIM_MOBA
W_RET_QK = N_HEADS_RET * HEAD_DIM_RET_QK
W_RET_V = N_HEADS_RET * HEAD_DIM_RET_V
IN_SIZES = (W_MOBA, W_MOBA, W_MOBA, W_RET_QK, W_RET_QK, W_RET_V, W_RET_V, D_MODEL, D_MODEL)
N_IN = sum(IN_SIZES)

kernel_name = "hybrid_moba_retention_hmoe_block"


def rmsnorm(x, w):
    xf = x.astype(jnp.float32)
    xf = xf * lax.rsqrt(jnp.mean(xf * xf, axis=-1, keepdims=True) + RMS_EPS)
    return xf.astype(x.dtype) * w


def to_heads(t, n_heads):
    b, s, _ = t.shape
    return t.reshape(b, s, n_heads, -1).transpose(0, 2, 1, 3)


def moba_attention(q, k, v):
    B, H, S, hd = q.shape
    nb = -(-S // MOBA_BLOCK)
    pad = nb * MOBA_BLOCK - S
    kp = jnp.pad(k, ((0, 0), (0, 0), (0, pad), (0, 0)))
    vp = jnp.pad(v, ((0, 0), (0, 0), (0, pad), (0, 0)))
    kb = kp.reshape(B, H, nb, MOBA_BLOCK, hd)
    vb = vp.reshape(B, H, nb, MOBA_BLOCK, hd)
    scale = hd ** -0.5
    k_mean = jnp.mean(kb.astype(jnp.float32), axis=3)
    gate = jnp.einsum('bhsd,bhnd->bhsn', q.astype(jnp.float32), k_mean)
    q_block = jnp.arange(S) // MOBA_BLOCK
    past = jnp.arange(nb)[None, :] < q_block[:, None]
    gate = jnp.where(past, gate, -jnp.inf)
    n_sel = min(MOBA_TOPK, nb)
    _, sel = lax.top_k(gate, n_sel)
    valid = sel < q_block[None, None, :, None]

    nq = S // MOBA_Q_CHUNK
    qc = q.reshape(B, H, nq, MOBA_Q_CHUNK, hd).transpose(2, 0, 1, 3, 4)
    selc = sel.reshape(B, H, nq, MOBA_Q_CHUNK, n_sel).transpose(2, 0, 1, 3, 4)
    validc = valid.reshape(B, H, nq, MOBA_Q_CHUNK, n_sel).transpose(2, 0, 1, 3, 4)
    bi = jnp.arange(B)[:, None, None, None]
    hi = jnp.arange(H)[None, :, None, None]
    n_sel_keys = n_sel * MOBA_BLOCK

    def one_chunk(args):
        c, q_c, sel_c, valid_c = args
        start = c * MOBA_Q_CHUNK
        blk = start // MOBA_BLOCK
        q_pos = start + jnp.arange(MOBA_Q_CHUNK)
        k_sel = kb[bi, hi, sel_c]
        v_sel = vb[bi, hi, sel_c]
        s_sel = jnp.einsum('bhqd,bhqnkd->bhqnk', q_c, k_sel).astype(jnp.float32) * scale
        s_sel = jnp.where(valid_c[..., None], s_sel, NEG_BIG).reshape(B, H, MOBA_Q_CHUNK, n_sel_keys)
        k_own = lax.dynamic_slice_in_dim(kp, blk * MOBA_BLOCK, MOBA_BLOCK, axis=2)
        v_own = lax.dynamic_slice_in_dim(vp, blk * MOBA_BLOCK, MOBA_BLOCK, axis=2)
        s_own = jnp.einsum('bhqd,bhkd->bhqk', q_c, k_own).astype(jnp.float32) * scale
        k_pos = blk * MOBA_BLOCK + jnp.arange(MOBA_BLOCK)
        s_own = jnp.where(k_pos[None, :] <= q_pos[:, None], s_own, NEG_BIG)
        p = jax.nn.softmax(jnp.concatenate([s_sel, s_own], axis=-1), axis=-1)
        p_sel = p[..., :n_sel_keys].reshape(B, H, MOBA_Q_CHUNK, n_sel, MOBA_BLOCK)
        p_own = p[..., n_sel_keys:]
        out = (jnp.einsum('bhqnk,bhqnkd->bhqd', p_sel, v_sel.astype(jnp.float32))
               + jnp.einsum('bhqk,bhkd->bhqd', p_own, v_own.astype(jnp.float32)))
        return out.astype(q.dtype)

    out = lax.map(one_chunk, (jnp.arange(nq), qc, selc, validc))
    return out.transpose(1, 2, 0, 3, 4).reshape(B, H, S, hd)


def rotary(x, pos):
    half = x.shape[-1] // 2
    inv = ROPE_BASE ** (-jnp.arange(half, dtype=jnp.float32) / half)
    ang = pos.astype(jnp.float32)[:, None] * inv[None, :]
    cos, sin = jnp.cos(ang), jnp.sin(ang)
    x1 = x[..., :half].astype(jnp.float32)
    x2 = x[..., half:].astype(jnp.float32)
    return jnp.concatenate([x1 * cos - x2 * sin, x1 * sin + x2 * cos], axis=-1).astype(x.dtype)


def retention(q, k, v):
    B, H, S, dk = q.shape
    dv = v.shape[-1]
    C = RET_CHUNK
    nc = S // C
    log_gamma = jnp.log(1.0 - 2.0 ** (-5.0 - jnp.arange(H, dtype=jnp.float32)))
    idx = jnp.arange(C, dtype=jnp.float32)
    diff = idx[:, None] - idx[None, :]
    decay_in = jnp.where(diff >= 0, jnp.exp(log_gamma[:, None, None] * jnp.maximum(diff, 0.0)), 0.0)
    qc = q.astype(jnp.float32).reshape(B, H, nc, C, dk)
    kc = k.astype(jnp.float32).reshape(B, H, nc, C, dk)
    vc = v.astype(jnp.float32).reshape(B, H, nc, C, dv)
    scores = jnp.einsum('bhnid,bhnjd->bhnij', qc, kc) * decay_in[None, :, None]
    o_inner = jnp.einsum('bhnij,bhnjv->bhniv', scores, vc)
    zeta = jnp.exp(log_gamma[:, None] * (C - 1.0 - idx)[None, :])
    u = jnp.einsum('bhnjd,bhnjv->nbhdv', kc * zeta[None, :, None, :, None], vc)
    chunk_decay = jnp.exp(log_gamma * C)[None, :, None, None]

    def step(state, u_n):
        return chunk_decay * state + u_n, state

    _, r_prev = lax.scan(step, jnp.zeros((B, H, dk, dv), jnp.float32), u)
    xi = jnp.exp(log_gamma[:, None] * (idx + 1.0)[None, :])
    o_cross = jnp.einsum('bhnid,nbhdv->bhniv', qc * xi[None, :, None, :, None], r_prev)
    return (o_inner + o_cross).reshape(B, H, S, dv)


def head_group_norm(y):
    mu = jnp.mean(y, axis=-1, keepdims=True)
    var = jnp.mean(jnp.square(y - mu), axis=-1, keepdims=True)
    yn = (y - mu) * lax.rsqrt(var + GN_EPS)
    B, H, S, dv = y.shape
    return yn.transpose(0, 2, 1, 3).reshape(B, S, H * dv)


def hierarchical_moe(h, w_rg, b_rg, w_re, b_re, w_gate, w_up, w_down):
    hf = h.astype(jnp.float32)
    g_logits = hf @ w_rg.astype(jnp.float32) + b_rg.astype(jnp.float32)
    g_prob = jax.nn.softmax(g_logits, axis=-1)
    g_onehot = jax.nn.one_hot(jnp.argmax(g_logits, axis=-1), N_GROUPS, dtype=jnp.float32)
    g_weight = jnp.sum(g_prob * g_onehot, axis=-1, keepdims=True)
    e_logits_all = jnp.einsum('td,gde->tge', hf, w_re.astype(jnp.float32)) + b_re.astype(jnp.float32)
    e_logits = jnp.einsum('tge,tg->te', e_logits_all, g_onehot)
    e_prob = jax.nn.softmax(e_logits, axis=-1)
    top_p, top_i = lax.top_k(e_prob, EXPERT_TOPK)
    top_p = top_p / jnp.sum(top_p, axis=-1, keepdims=True)
    within = jnp.sum(jax.nn.one_hot(top_i, EXPERTS_PER_GROUP, dtype=jnp.float32) * top_p[..., None], axis=1)
    comb = ((g_weight * g_onehot)[:, :, None] * within[:, None, :]).reshape(h.shape[0], N_EXPERTS).astype(h.dtype)
    y = jnp.zeros_like(h)
    for e in range(N_EXPERTS):
        act = jax.nn.silu(h @ w_gate[e]) * (h @ w_up[e])
        y = y + comb[:, e:e + 1] * (act @ w_down[e])
    return y


def setup_inputs(seed: int = 0) -> dict:
    key = jax.random.key(seed)
    ks = jax.random.split(key, 16)
    f = jnp.float32
    n = jax.random.normal
    return {
        "x": n(ks[0], (BATCH, SEQ, D_MODEL), f),
        "norm_mix_w": 1.0 + 0.05 * n(ks[1], (DEPTH, D_MODEL), f),
        "w_in": n(ks[2], (DEPTH, D_MODEL, N_IN), f) * D_MODEL ** -0.5,
        "ret_gn_w": 1.0 + 0.05 * n(ks[3], (DEPTH, W_RET_V), f),
        "w_branch_moba": n(ks[4], (DEPTH, W_MOBA, D_MODEL), f) * W_MOBA ** -0.5,
        "w_branch_ret": n(ks[5], (DEPTH, W_RET_V, D_MODEL), f) * W_RET_V ** -0.5,
        "w_out": n(ks[6], (DEPTH, D_MODEL, D_MODEL), f) * D_MODEL ** -0.5,
        "norm_ffn_w": 1.0 + 0.05 * n(ks[7], (DEPTH, D_MODEL), f),
        "w_router_group": n(ks[8], (DEPTH, D_MODEL, N_GROUPS), f) * D_MODEL ** -0.5,
        "b_router_group": 0.01 * n(ks[9], (DEPTH, N_GROUPS), f),
        "w_router_expert": n(ks[10], (DEPTH, N_GROUPS, D_MODEL, EXPERTS_PER_GROUP), f) * D_MODEL ** -0.5,
        "b_router_expert": 0.01 * n(ks[11], (DEPTH, N_GROUPS, EXPERTS_PER_GROUP), f),
        "w_expert_gate": n(ks[12], (DEPTH, N_EXPERTS, D_MODEL, D_EXPERT), f) * D_MODEL ** -0.5,
        "w_expert_up": n(ks[13], (DEPTH, N_EXPERTS, D_MODEL, D_EXPERT), f) * D_MODEL ** -0.5,
        "w_expert_down": n(ks[14], (DEPTH, N_EXPERTS, D_EXPERT, D_MODEL), f) * D_EXPERT ** -0.5,
        "norm_final_w": 1.0 + 0.05 * n(ks[15], (D_MODEL,), f),
    }


def reference(x, norm_mix_w, w_in, ret_gn_w, w_branch_moba, w_branch_ret, w_out, norm_ffn_w,
              w_router_group, b_router_group, w_router_expert, b_router_expert,
              w_expert_gate, w_expert_up, w_expert_down, norm_final_w):
    B, S, D = x.shape
    pos = jnp.arange(S)
    split_points = np.cumsum(IN_SIZES)[:-1].tolist()
    for l in range(DEPTH):
        h = rmsnorm(x, norm_mix_w[l])
        proj = h @ w_in[l]
        mq, mk, mv, rq, rk, rv, rg, ga, gr = jnp.split(proj, split_points, axis=-1)
        o_a = moba_attention(to_heads(mq, N_HEADS_MOBA), to_heads(mk, N_HEADS_MOBA), to_heads(mv, N_HEADS_MOBA))
        o_a = o_a.transpose(0, 2, 1, 3).reshape(B, S, W_MOBA)
        q_r = rotary(to_heads(rq, N_HEADS_RET), pos)
        k_r = rotary(to_heads(rk, N_HEADS_RET), pos) * (HEAD_DIM_RET_QK ** -0.5)
        y_r = retention(q_r, k_r, to_heads(rv, N_HEADS_RET))
        y_r = head_group_norm(y_r).astype(x.dtype) * ret_gn_w[l]
        y_r = jax.nn.silu(rg) * y_r
        mix = jax.nn.sigmoid(ga) * (o_a @ w_branch_moba[l]) + jax.nn.sigmoid(gr) * (y_r @ w_branch_ret[l])
        x = x + mix @ w_out[l]
        h = rmsnorm(x, norm_ffn_w[l]).reshape(B * S, D)
        y = hierarchical_moe(h, w_router_group[l], b_router_group[l], w_router_expert[l], b_router_expert[l],
                             w_expert_gate[l], w_expert_up[l], w_expert_down[l])
        x = x + y.reshape(B, S, D)
    return rmsnorm(x, norm_final_w)
```

```python
from contextlib import ExitStack

import numpy as np
import ml_dtypes
import concourse.bass as bass
import concourse.mybir as mybir
from concourse.bass_utils import run_bass_kernel_spmd

F32 = mybir.dt.float32
BF16 = mybir.dt.bfloat16
AF = mybir.ActivationFunctionType
ALU = mybir.AluOpType

D = 2048
NSLOT = 4096
NOWN = 1024
OWN0 = NSLOT - NOWN
NH = 8
NEXP = 32
DEXP = 512
SCALE = 128.0 ** -0.5
NEG = -30000.0
RMS_EPS = 1e-6
GN_EPS = 1e-6


DECL = []
UNIQ = [0]


class _Cut(Exception):
    pass


CUT = [0]


class Buf:
    __slots__ = ("name", "w", "r", "excl")

    def __init__(self, name, excl=False):
        self.name = name
        self.w = None
        self.r = {}
        self.excl = excl


class Sched:
    ENG = ("pe", "dve", "act", "pool", "sp")

    def __init__(self, nc):
        self.nc = nc
        self.eng = {"pe": nc.tensor, "dve": nc.vector, "act": nc.scalar,
                    "pool": nc.gpsimd, "sp": nc.sync}
        self.sem = {e: nc.alloc_semaphore("s_" + e) for e in self.ENG}
        self.cnt = {e: 0 for e in self.ENG}
        self.waited = {}
        self.dsem = {}
        self.dcnt = {}
        self.ninst = 0

    def _wait(self, x, tok):
        if tok is None:
            return
        if tok[0] == "dma":
            _, slot, val = tok
            key = (x, "dma", slot)
            if self.waited.get(key, 0) >= val:
                return
            self.eng[x].wait_ge(self.dsem[slot], val)
            self.waited[key] = val
        else:
            e, val = tok
            if e == x and x == "pe":
                return
            key = (x, e)
            if self.waited.get(key, 0) >= val:
                return
            self.eng[x].wait_ge(self.sem[e], val)
            self.waited[key] = val

    def _deps(self, x, reads, writes):
        for b in reads:
            self._wait(x, b.w)
            if b.excl:
                for t in b.r.values():
                    if t[0] != x:
                        self._wait(x, t)
        for b in writes:
            self._wait(x, b.w)
            for t in b.r.values():
                self._wait(x, t)

    @staticmethod
    def _commit(tok, reads, writes):
        k = tok[:-1]
        for b in reads:
            b.r[k] = tok
        for b in writes:
            b.w = tok
            b.r = {}

    def op(self, x, fn, reads=(), writes=()):
        self._deps(x, reads, writes)
        ins = fn(self.eng[x])
        self.cnt[x] += 1
        ins.then_inc(self.sem[x], 1)
        tok = (x, self.cnt[x])
        self._commit(tok, reads, writes)
        self.ninst += 1
        return tok

    def dma(self, q, slot, out, in_, reads=(), writes=()):
        if slot not in self.dsem:
            self.dsem[slot] = self.nc.alloc_semaphore("d_" + slot)
            self.dcnt[slot] = 0
        self._deps(q, reads, writes)
        ins = self.eng[q].dma_start(out=out, in_=in_)
        self.dcnt[slot] += 16
        ins.then_inc(self.dsem[slot], 16)
        tok = ("dma", slot, self.dcnt[slot])
        self._commit(tok, reads, writes)
        self.ninst += 1
        return tok

    def barrier(self):
        for x in self.ENG:
            for e in self.ENG:
                if e != x and self.cnt[e] > 0:
                    self._wait(x, (e, self.cnt[e]))
            for slot, v in self.dcnt.items():
                self._wait(x, ("dma", slot, v))


def build(upto="full", debug=False, nheads=NH):
    nc = bass.Bass("TRN2", target_bir_lowering=False)
    S = Sched(nc)

    DECL.clear()
    early = upto in ("p0", "p1")

    def din(name, shape, dt=F32):
        DECL.append(name)
        return nc.dram_tensor(name, list(shape), dt, kind="ExternalInput")

    xs = din("xs", [NSLOT, D]).ap()
    w_in = din("w_in", [D, 13312]).ap()
    nw1T_d = din("nw1T", [128, 16]).ap()
    nw2T_d = din("nw2T", [128, 16]).ap()
    nwf_d = din("nwf", [1, D])
    gnw_d = din("gnw", [1, D])
    rot_d = din("rot", [128, 2, NSLOT]).ap()
    dT_d = din("dT", [128, NH, 128]).ap()
    cols_d = din("cols", [128, 3, NH]).ap()
    pmask_d = din("pmask", [128, 4, 16]).ap()
    selhot_d = din("selhot", [128, 16, 128], BF16).ap()
    ident_d = din("ident", [128, 128], BF16).ap()
    dmask_d = din("dmask", [128, 256], BF16).ap()
    if not early:
        wbm = din("wbm", [1024, D]).ap()
        wbr = din("wbr", [D, D]).ap()
        wout = din("wout", [D, D]).ap()
        wr36_d = din("wr36", [D, 36]).ap()
        br36_d = din("br36", [1, 36])
        weg = din("weg", [NEXP, D, DEXP]).ap()
        weu = din("weu", [NEXP, D, DEXP]).ap()
        wed = din("wed", [NEXP, DEXP, D]).ap()
        out = nc.dram_tensor("out", [NOWN, D], F32, kind="ExternalOutput").ap()

    hT_scr = nc.dram_tensor("hT_scr", [D, NSLOT], BF16).ap()
    oy_scr = nc.dram_tensor("oy_scr", [3072, NOWN], BF16).ap()
    hT_v = hT_scr.rearrange("(c p) s -> p c s", p=128)
    b_scr = [Buf(f"scr{g}") for g in range(8)]
    b_oy = Buf("oy")

    dbg = {}

    def cut(k):
        if CUT[0] == k:
            S.barrier()
            dc = dbg_out('dbg_cut', [128, 16])
            S.dma('sp', 'dcut', dc[:, :], nw1T[:, :])
            S.barrier()
            raise _Cut()

    def dbg_out(name, shape, dt=F32):
        t = nc.dram_tensor(name, list(shape), dt, kind="ExternalOutput").ap()
        dbg[name] = t
        return t

    pb = [nc.alloc_psum_tensor(f"pb{i}", [128, 512], F32) for i in range(8)]
    b_pb = [Buf(f"pb{i}", excl=True) for i in range(8)]

    def pbf(i):
        return pb[i][:, :].bitcast(BF16)

    def sb(name, shape, dt=F32):
        return nc.alloc_sbuf_tensor("s_" + name, list(shape), dt)

    ident = sb("ident", [128, 128], BF16); b_ident = Buf("ident")
    S.dma("sp", "c0_1", ident[:, :], ident_d[:, :], writes=[b_ident])
    nw1T = sb("nw1T", [128, 16]); nw2T = sb("nw2T", [128, 16]); b_nw = Buf("nw")
    S.dma("sp", "c0_2", nw1T[:, :], nw1T_d[:, :], writes=[b_nw])
    S.dma("sp", "c0_3", nw2T[:, :], nw2T_d[:, :], writes=[b_nw])

    def bc_last(t, n_mid, w, off=0):
        return bass.AP(t, off, [[t.shape[1], 128], [1, n_mid], [0, w]])

    def rms_tile(src_ap, b_src, junk, b_junk, st, b_st, j):
        S.op("act", lambda e: e.activation(out=junk[:, :], in_=src_ap, func=AF.Square,
                                           accum_out=st[:, 2 * j:2 * j + 1]),
             reads=[b_src], writes=[b_junk, b_st])
        S.op("act", lambda e: e.activation(out=st[:, 2 * j:2 * j + 1], in_=st[:, 2 * j:2 * j + 1],
                                           func=AF.Sqrt, scale=1.0 / D, bias=RMS_EPS),
             reads=[b_st], writes=[b_st])
        S.op("dve", lambda e: e.reciprocal(out=st[:, 2 * j + 1:2 * j + 2], in_=st[:, 2 * j:2 * j + 1]),
             reads=[b_st], writes=[b_st])

    tp_cnt = [0]

    def transpose_tile(hb, b_hb, dst_fn, b_dst_fn, banks=(0, 1, 2, 3, 4, 5, 6, 7)):
        for cq in range(4):
            bk = banks[tp_cnt[0] % len(banks)]
            tp_cnt[0] += 1
            tp = pbf(bk)[:, 0:512].rearrange("p (a b) -> p a b", b=128)
            for ci in range(4):
                c = cq * 4 + ci
                S.op("pe", lambda e: e.transpose(out=tp[:, ci, :], in_=hb[:, c * 128:(c + 1) * 128],
                                                 identity=ident[:, :]),
                     reads=[b_hb, b_ident], writes=[b_pb[bk]])
            if cq % 2 == 0:
                S.op("act", lambda e: e.copy(out=dst_fn(cq), in_=tp), reads=[b_pb[bk]], writes=[b_dst_fn(cq)])
            else:
                S.op("dve", lambda e: e.tensor_copy(out=dst_fn(cq), in_=tp), reads=[b_pb[bk]], writes=[b_dst_fn(cq)])

    es01 = ExitStack()

    def tmp01(name, shape, dt=F32):
        UNIQ[0] += 1
        return es01.enter_context(nc.sbuf_tensor(f"t{UNIQ[0]}_" + name, list(shape), dt))
    W1b = tmp01("W1b", [128, 16, 1152], BF16); b_W1b = Buf("W1b")
    stg = [tmp01(f"stg{i}", [128, 16, 128]) for i in range(3)]; b_stg = [Buf(f"stg{i}") for i in range(3)]
    colmap = [(0, 0), (1024, 128), (3072, 256), (4096, 384), (2048, 512)]

    def load_w1(h, eng="pool"):
        blocks = [(base + h * 128, off) for base, off in colmap]
        blocks += [(5120 + h * 256, 640), (5120 + h * 256 + 128, 768), (7168 + h * 256, 896), (7168 + h * 256 + 128, 1024)]
        for bi, (c0, off) in enumerate(blocks):
            k = (h * 9 + bi) % 3
            S.dma("sp" if h == 0 else "pool", f"ws{k}" if h == 0 else f"wp{k}", stg[k][:, :, :], w_in[:, c0:c0 + 128].rearrange("(c p) n -> p c n", p=128),
                  writes=[b_stg[k]])
            S.op(eng, lambda e: e.tensor_tensor(out=W1b[:, :, off:off + 128], in0=stg[k][:, :, :],
                                                in1=bc_last(nw1T, 16, 128), op=ALU.mult),
                 reads=[b_stg[k], b_nw], writes=[b_W1b])

    load_w1(0)
    with ExitStack() as es:
        def tmp(name, shape, dt=F32):
            UNIQ[0] += 1
            return es.enter_context(nc.sbuf_tensor(f"t{UNIQ[0]}_" + name, list(shape), dt))
        xst = [tmp(f"xst{i}", [128, D]) for i in range(3)]; b_xst = [Buf(f"xst{i}") for i in range(3)]
        junk = tmp("junk0", [128, D], BF16); b_junk = Buf("junk")
        hb = [tmp(f"hb{i}", [128, D], BF16) for i in range(2)]; b_hb = [Buf(f"hb{i}") for i in range(2)]
        hTg = [tmp(f"hTg{i}", [128, 16, 512], BF16) for i in range(2)]
        b_hTgp = [[Buf(f"hTg{i}_{j}") for j in range(16)] for i in range(2)]
        st = tmp("st0", [128, 8]); b_st = [Buf(f"st{i}") for i in range(4)]

        def p0_a(t):
            k = t % 3
            j = t % 4
            hk = t % 2
            S.dma("sp", f"x{k}", xst[k][:, :], xs[t * 128:(t + 1) * 128, :], writes=[b_xst[k]])
            rms_tile(xst[k][:, :], b_xst[k], junk, b_junk, st, b_st[j], j)
            S.op("dve", lambda e: e.tensor_scalar(out=hb[hk][:, :], in0=xst[k][:, :],
                                                  scalar1=st[:, 2 * j + 1:2 * j + 2], scalar2=None, op0=ALU.mult),
                 reads=[b_xst[k], b_st[j]], writes=[b_hb[hk]])

        def p0_b(t):
            g, tl = divmod(t, 4)
            hk = t % 2
            gi = g % 2
            transpose_tile(hb[hk], b_hb[hk],
                           lambda cq: hTg[gi][:, 4 * cq:4 * cq + 4, tl * 128:(tl + 1) * 128],
                           lambda cq: b_hTgp[gi][tl * 4 + cq])
            if tl == 3:
                S.dma("pool", f"hs{gi}", hT_v[:, :, g * 512:(g + 1) * 512], hTg[gi][:, :, :],
                      reads=b_hTgp[gi], writes=[b_scr[g]])
        NT0 = NSLOT // 128
        p0_a(0)
        for t in range(NT0):
            if t + 1 < NT0:
                p0_a(t + 1)
            p0_b(t)
        S.barrier()

    if upto == "p0":
        d = dbg_out("dbg_hT", [D, NSLOT], BF16)
        with ExitStack() as es:
            t_ = es.enter_context(nc.sbuf_tensor("dbgt", [128, 16, 512], BF16)); b_t = Buf("dbgt")
            for g in range(8):
                S.dma("sp", "dl", t_[:, :, :], hT_v[:, :, g * 512:(g + 1) * 512], reads=[b_scr[g]], writes=[b_t])
                S.dma("sp", "ds", d.rearrange("(c p) s -> p c s", p=128)[:, :, g * 512:(g + 1) * 512], t_[:, :, :], reads=[b_t])
            S.barrier()
        return nc, dbg

    try:
      with ExitStack() as es:
        def tmp(name, shape, dt=F32):
            UNIQ[0] += 1
            return es.enter_context(nc.sbuf_tensor(f"t{UNIQ[0]}_" + name, list(shape), dt))
        rotc = None
        dT = tmp("dT", [128, NH, 128]); cols = tmp("cols", [128, 3, NH]); pmask = tmp("pmask", [128, 4, 16])
        selhot = tmp("selhot", [128, 16, 128], BF16); dmask = tmp("dmask", [128, 256], BF16)
        gnwB = tmp("gnwB", [128, D])
        b_c1 = Buf("c1")
        S.dma("sp", "c1_4", dT[:, :, :], dT_d[:, :, :], writes=[b_c1])
        S.dma("sp", "c1_5", cols[:, :, :], cols_d[:, :, :], writes=[b_c1])
        S.dma("sp", "c1_6", pmask[:, :, :], pmask_d[:, :, :], writes=[b_c1])
        S.dma("sp", "c1_7", selhot[:, :, :], selhot_d[:, :, :], writes=[b_c1])
        S.dma("sp", "c1_8", dmask[:, :], dmask_d[:, :], writes=[b_c1])
        S.dma("sp", "c1_9", gnwB[:, :], bass.AP(gnw_d, 0, [[0, 128], [1, D]]), writes=[b_c1])

        hTg = [tmp(f"hTg{i}", [128, 16, 512], BF16) for i in range(2)]; b_hTg = [Buf(f"hTg{i}") for i in range(2)]
        rt = [tmp(f"rt{i}", [128, 2, 512]) for i in range(2)]; b_rt = [Buf(f"rt{i}") for i in range(2)]
        KT = tmp("KT", [128, NSLOT], BF16); b_KT = [Buf(f"KT{g}") for g in range(8)]
        V = tmp("V", [128, 32, 129], BF16); b_V = [Buf(f"V{g}") for g in range(8)]
        QT = tmp("QT", [128, NOWN], BF16); b_QT = Buf("QT")
        QRT = tmp("QRT", [128, NOWN], BF16); b_QRT = Buf("QRT")
        KRT = tmp("KRT", [128, NOWN], BF16); b_KRT = Buf("KRT")
        KRg = [tmp(f"KRg{i}", [128, 512], BF16) for i in range(2)]; b_KRg = [Buf(f"KRg{i}") for i in range(2)]
        KZ = [tmp(f"KZ{i}", [128, 4, 128], BF16) for i in range(2)]; b_KZ = [Buf(f"KZ{i}") for i in range(2)]
        RVt = [tmp(f"RVt{i}", [128, 256], BF16) for i in range(8)]; b_RVt = [Buf(f"RVt{i}") for i in range(8)]
        RVo = tmp("RVo", [128, 8, 256], BF16); b_RVo = [Buf(f"RVo{i}") for i in range(8)]
        SGo = tmp("SGo", [128, 8, 256], BF16); b_SGo = [Buf(f"SGo{i}") for i in range(8)]
        sil = [tmp(f"sil{i}", [128, 256]) for i in range(2)]; b_sil = [Buf(f"sil{i}") for i in range(2)]
        Rst = tmp("Rst", [128, 256]); b_R = Buf("R")
        Rb = tmp("Rb", [128, 8, 256], BF16); b_Rb = [Buf(f"Rb{i}") for i in range(8)]
        kms = tmp("kms", [128, 16]); b_kms = Buf("kms")
        kmb = tmp("kmb", [128, 16], BF16); b_kmb = Buf("kmb")
        maskT = tmp("maskT", [128, NOWN], BF16); b_maskT = Buf("maskT")
        selbp8 = tmp("selbp8", [128, 8, 128], BF16); b_selbp8 = [Buf(f"selbp8_{i}") for i in range(8)]
        gsm8 = tmp("gsm8", [128, 8, 48]); b_gsm8 = [Buf(f"gsm8_{i}") for i in range(8)]
        PT = [tmp(f"PT{i}", [128, 512], BF16) for i in range(3)]; b_PT = [Buf(f"PT{i}") for i in range(3)]
        t1 = [tmp(f"t1_{i}", [128, 512]) for i in range(2)]; b_t1 = [Buf(f"t1_{i}") for i in range(2)]
        t2 = [tmp(f"t2_{i}", [128, 512]) for i in range(2)]; b_t2 = [Buf(f"t2_{i}") for i in range(2)]
        OAt = [tmp(f"OAt{i}", [128, 128], BF16) for i in range(2)]; b_OAt = [Buf(f"OAt{i}") for i in range(2)]
        rec = tmp("rec", [128, 4]); b_rec = [Buf(f"rec{i}") for i in range(2)]
        OATs = tmp("OATs", [128, NOWN], BF16); b_OATs = Buf("OATs")
        YRTs = tmp("YRTs", [128, 2, NOWN], BF16); b_YRTs = Buf("YRTs")
        STb8 = tmp("STb8", [128, 8, 128], BF16); b_STb8 = [Buf(f"STb8_{i}") for i in range(8)]
        tcx = [tmp(f"tcx{i}", [128, 256]) for i in range(2)]; b_tcx = [Buf(f"tcx{i}") for i in range(2)]
        osb8 = tmp("osb8", [128, 8, 256]); b_osb8 = [Buf(f"osb8_{i}") for i in range(8)]
        gst8 = tmp("gst8", [128, 8, 12]); b_gst8 = [Buf(f"gst8_{i}") for i in range(8)]
        yb8 = tmp("yb8", [128, 8, 256], BF16); b_yb8 = [Buf(f"yb8_{i}") for i in range(8)]

        S.op("pool", lambda e: e.memset(V[:, :, :], 1.0), writes=b_V)
        S.op("pool", lambda e: e.memset(maskT[:, :], 0.0), writes=[b_maskT])
        S.op("pool", lambda e: e.memset(selbp8[:, :, :], 0.0), writes=b_selbp8)

        pfc = [0]; ptc = [0]; trc = [0]; rc = [0]

        def rotary_evac(bk, dst_ap, b_dst, rti, b_rti):
            r = rc[0] % 2
            rc[0] += 1
            S.op("dve", lambda e: e.tensor_tensor(out=t1[r][:, :], in0=pb[bk][:, :], in1=rti[:, 0, :], op=ALU.mult),
                 reads=[b_pb[bk], b_rti], writes=[b_t1[r]])
            S.op("dve", lambda e: e.tensor_tensor(out=t2[r][0:64, :], in0=pb[bk][64:128, :], in1=rti[0:64, 1, :], op=ALU.mult),
                 reads=[b_pb[bk], b_rti], writes=[b_t2[r]])
            S.op("dve", lambda e: e.tensor_tensor(out=t2[r][64:128, :], in0=pb[bk][0:64, :], in1=rti[64:128, 1, :], op=ALU.mult),
                 reads=[b_pb[bk], b_rti], writes=[b_t2[r]])
            S.op("pool", lambda e: e.tensor_tensor(out=dst_ap, in0=t1[r][:, :], in1=t2[r][:, :], op=ALU.add),
                 reads=[b_t1[r], b_t2[r]], writes=[b_dst])

        def proj_fm(col, hti, b_hti):
            bk = (0, 1, 6, 7)[pfc[0] % 4]
            pfc[0] += 1
            for c in range(16):
                S.op("pe", lambda e: e.matmul(pb[bk][:, :], lhsT=W1b[:, c, col:col + 128], rhs=hti[:, c, :],
                                              start=(c == 0), stop=(c == 15)),
                     reads=[b_W1b, b_hti], writes=[b_pb[bk]])
            return bk

        def retA(h):
            for n in range(8):
                cs = slice(n * 128, (n + 1) * 128)
                bk = stbanks[stc[0] % 3]
                stc[0] += 1
                S.op("pe", lambda e: e.matmul(pb[bk][:, 0:128], lhsT=KRT[:, cs], rhs=QRT[:, cs], start=True, stop=True),
                     reads=[b_KRT, b_QRT], writes=[b_pb[bk]])
                S.op("dve", lambda e: e.tensor_tensor(out=STb8[:, n, :], in0=pb[bk][:, 0:128], in1=dT[:, h, :], op=ALU.mult),
                     reads=[b_pb[bk], b_c1], writes=[b_STb8[n]])
            for n in range(8):
                cs = slice(n * 128, (n + 1) * 128)
                r = n % 2
                bi_, bc_ = (2, 3) if r == 0 else (7, 1)
                S.op("pe", lambda e: e.matmul(pb[bc_][:, 0:256], lhsT=QRT[:, cs], rhs=Rb[:, n, :], start=True, stop=True),
                     reads=[b_QRT, b_Rb[n]], writes=[b_pb[bc_]])
                S.op("pe", lambda e: e.matmul(pb[bi_][:, 0:256], lhsT=STb8[:, n, :], rhs=RVo[:, n, :], start=True, stop=True),
                     reads=[b_STb8[n], b_RVo[n]], writes=[b_pb[bi_]])
                S.op("act", lambda e: e.activation(out=tcx[r][:, :], in_=pb[bc_][:, 0:256], func=AF.Copy, scale=cols[:, 0, h:h + 1]),
                     reads=[b_pb[bc_], b_c1], writes=[b_tcx[r]])
                S.op("dve", lambda e: e.tensor_tensor(out=osb8[:, n, :], in0=pb[bi_][:, 0:256], in1=tcx[r][:, :], op=ALU.add),
                     reads=[b_pb[bi_], b_tcx[r]], writes=[b_osb8[n]])

        def retB(h):
            for n in range(8):
                S.op("dve", lambda e: e.bn_stats(out=gst8[:, n, 0:6], in_=osb8[:, n, :]), reads=[b_osb8[n]], writes=[b_gst8[n]])
            for n in range(8):
                S.op("dve", lambda e: e.bn_aggr(out=gst8[:, n, 6:8], in_=gst8[:, n, 0:6]), reads=[b_gst8[n]], writes=[b_gst8[n]])
            S.op("act", lambda e: e.activation(out=gst8[:, :, 8], in_=gst8[:, :, 7], func=AF.Sqrt, scale=1.0, bias=GN_EPS),
                 reads=b_gst8, writes=b_gst8)
            S.op("dve", lambda e: e.reciprocal(out=gst8[:, :, 9], in_=gst8[:, :, 8]), reads=b_gst8, writes=b_gst8)
            for n in range(8):
                S.op("dve", lambda e: e.tensor_scalar(out=osb8[:, n, :], in0=osb8[:, n, :], scalar1=gst8[:, n, 6:7],
                                                      scalar2=gst8[:, n, 9:10], op0=ALU.subtract, op1=ALU.mult),
                     reads=[b_osb8[n], b_gst8[n]], writes=[b_osb8[n]])
                S.op("pool", lambda e: e.tensor_tensor(out=yb8[:, n, :], in0=osb8[:, n, :], in1=SGo[:, n, :], op=ALU.mult),
                     reads=[b_osb8[n], b_SGo[n]], writes=[b_yb8[n]])

        def retC(h):
            for n in range(8):
                cs = slice(n * 128, (n + 1) * 128)
                tb = 4 + trc[0] % 2
                trc[0] += 1
                tpv = pbf(tb)[:, 0:256].rearrange("p (a b) -> p a b", b=128)
                for k2 in range(2):
                    S.op("pe", lambda e: e.transpose(out=tpv[:, k2, :], in_=yb8[:, n, k2 * 128:(k2 + 1) * 128], identity=ident[:, :]),
                         reads=[b_yb8[n], b_ident], writes=[b_pb[tb]])
                S.op("act", lambda e: e.copy(out=YRTs[:, :, cs], in_=tpv), reads=[b_pb[tb]], writes=[b_YRTs])
            S.dma("sp", "yr", oy_scr[1024 + h * 256:1024 + (h + 1) * 256, :].rearrange("(k p) t -> p k t", p=128),
                  YRTs[:, :, :], reads=[b_YRTs], writes=[b_oy])

        def load_grp(g):
            gi = g % 2
            S.dma("sp", f"hl{gi}", hTg[gi][:, :, :], hT_v[:, :, g * 512:(g + 1) * 512],
                  reads=[b_scr[g]], writes=[b_hTg[gi]])
            S.dma("sp", f"rl{gi}", rt[gi][:, :, :], rot_d[:, :, g * 512:(g + 1) * 512], writes=[b_rt[gi]])

        stbanks = (0, 1, 6)
        stc = [0]
        for h in range(nheads):
            cut(1)
            S.op("pool", lambda e: e.memset(Rst[:, :], 0.0), writes=[b_R])
            pendB = [None]
            for g in range(8):
                own = g >= 6
                gi = g % 2
                if not (h > 0 and g < 2):
                    load_grp(g)
                if h > 0 and g in (1, 2, 3):
                    (retA, retB, retC)[g - 1](h - 1)
                gs_ = slice(g * 512, (g + 1) * 512)
                os_ = slice((g - 6) * 512, (g - 5) * 512)
                cut(20)
                bk = proj_fm(128, hTg[gi], b_hTg[gi])
                cut(201)
                S.op("act", lambda e: e.copy(out=KT[:, gs_], in_=pb[bk][:, :]), reads=[b_pb[bk]], writes=[b_KT[g]])
                cut(202)
                S.op("dve", lambda e: e.tensor_reduce(out=kms[:, 2 * g:2 * g + 2],
                                                      in_=pb[bk][:, :].rearrange("p (a b) -> p a b", b=256),
                                                      axis=mybir.AxisListType.X, op=ALU.add),
                     reads=[b_pb[bk]], writes=[b_kms])
                cut(21)
                bk = proj_fm(384, hTg[gi], b_hTg[gi])
                kr = g % 2
                if own:
                    rotary_evac(bk, KRT[:, os_], b_KRT, rt[gi], b_rt[gi])
                    krsrc, b_krsrc = KRT, b_KRT
                    kroff = (g - 6) * 512
                else:
                    rotary_evac(bk, KRg[kr][:, :], b_KRg[kr], rt[gi], b_rt[gi])
                    krsrc, b_krsrc = KRg[kr], b_KRg[kr]
                    kroff = 0
                if own:
                    bk = proj_fm(0, hTg[gi], b_hTg[gi])
                    S.op("act", lambda e: e.copy(out=QT[:, os_], in_=pb[bk][:, :]), reads=[b_pb[bk]], writes=[b_QT])
                    bk = proj_fm(256, hTg[gi], b_hTg[gi])
                    rotary_evac(bk, QRT[:, os_], b_QRT, rt[gi], b_rt[gi])
                rvs = []
                for tl in range(4):
                    t = g * 4 + tl
                    n = t - 24
                    bk = (2, 3, 0, 1)[ptc[0] % 4]
                    ptc[0] += 1
                    for c in range(16):
                        S.op("pe", lambda e: e.matmul(pb[bk][:, 0:384], lhsT=hTg[gi][:, c, tl * 128:(tl + 1) * 128],
                                                      rhs=W1b[:, c, 512:896], start=(c == 0), stop=(c == 15)),
                             reads=[b_W1b, b_hTg[gi]], writes=[b_pb[bk]])
                    S.op("dve", lambda e: e.tensor_copy(out=V[:, t, 0:128], in_=pb[bk][:, 0:128]),
                         reads=[b_pb[bk]], writes=[b_V[g]])
                    if own:
                        rv_ap, b_rv = RVo[:, n, :], b_RVo[n]
                    else:
                        rv_ap, b_rv = RVt[t % 8][:, :], b_RVt[t % 8]
                    rvs.append((rv_ap, b_rv))
                    S.op("act", lambda e: e.copy(out=rv_ap, in_=pb[bk][:, 128:384]), reads=[b_pb[bk]], writes=[b_rv])
                    if own:
                        bk2 = (2, 3, 0, 1)[ptc[0] % 4]
                        ptc[0] += 1
                        for c in range(16):
                            S.op("pe", lambda e: e.matmul(pb[bk2][:, 0:256], lhsT=hTg[gi][:, c, tl * 128:(tl + 1) * 128],
                                                          rhs=W1b[:, c, 896:1152], start=(c == 0), stop=(c == 15)),
                                 reads=[b_W1b, b_hTg[gi]], writes=[b_pb[bk2]])
                        si = t % 2
                        S.op("act", lambda e: e.activation(out=sil[si][:, :], in_=pb[bk2][:, 0:256], func=AF.Silu),
                             reads=[b_pb[bk2]], writes=[b_sil[si]])
                        S.op("pool", lambda e: e.tensor_tensor(out=SGo[:, n, :], in0=sil[si][:, :],
                                                               in1=gnwB[:, h * 256:(h + 1) * 256], op=ALU.mult),
                             reads=[b_sil[si], b_c1], writes=[b_SGo[n]])
                def partB(g=g, own=own, kr=kr, krsrc=krsrc, b_krsrc=b_krsrc, kroff=kroff, rvs=rvs, h=h):
                    tb = 4 + trc[0] % 2
                    trc[0] += 1
                    tpv = pbf(tb)[:, 0:512].rearrange("p (a b) -> p a b", b=128)
                    for tl in range(4):
                        S.op("pe", lambda e: e.transpose(out=tpv[:, tl, :], in_=krsrc[:, kroff + tl * 128:kroff + (tl + 1) * 128],
                                                         identity=ident[:, :]),
                             reads=[b_krsrc, b_ident], writes=[b_pb[tb]])
                    S.op("act", lambda e: e.activation(out=KZ[kr][:, :, :], in_=tpv, func=AF.Copy, scale=cols[:, 1, h:h + 1]),
                         reads=[b_pb[tb], b_c1], writes=[b_KZ[kr]])
                    for tl in range(4):
                        t = g * 4 + tl
                        n = t - 24
                        rv_ap, b_rv = rvs[tl]
                        if own:
                            S.op("act", lambda e: e.copy(out=Rb[:, n, :], in_=Rst[:, :]), reads=[b_R], writes=[b_Rb[n]])
                        ub = (6, 7)[t % 2]
                        S.op("pe", lambda e: e.matmul(pb[ub][:, 0:256], lhsT=KZ[kr][:, tl, :], rhs=rv_ap, start=True, stop=True),
                             reads=[b_KZ[kr], b_rv], writes=[b_pb[ub]])
                        S.op("dve", lambda e: e.scalar_tensor_tensor(out=Rst[:, :], in0=Rst[:, :], scalar=cols[:, 2, h:h + 1],
                                                                     in1=pb[ub][:, 0:256], op0=ALU.mult, op1=ALU.add),
                             reads=[b_pb[ub], b_c1, b_R], writes=[b_R])

                if pendB[0] is not None:
                    pendB[0]()
                pendB[0] = partB
            pendB[0]()
            pendB[0] = None
            if h + 1 < nheads:
                load_grp(0)
                load_grp(1)
            cut(4)
            S.op("dve", lambda e: e.tensor_scalar(out=kmb[:, :], in0=kms[:, :], scalar1=1.0 / 256, scalar2=None, op0=ALU.mult),
                 reads=[b_kms], writes=[b_kmb])
            gbanks = (0, 1, 2, 3)
            for qt in range(8):
                i = qt // 2
                qs = slice(qt * 128, (qt + 1) * 128)
                gb = gbanks[qt % 4]
                S.op("pe", lambda e: e.matmul(pb[gb][:, 0:16], lhsT=QT[:, qs], rhs=kmb[:, :], start=True, stop=True),
                     reads=[b_QT, b_kmb], writes=[b_pb[gb]])
                S.op("dve", lambda e: e.tensor_tensor(out=gsm8[:, qt, 0:16], in0=pb[gb][:, 0:16], in1=pmask[:, i, :], op=ALU.add),
                     reads=[b_pb[gb], b_c1], writes=[b_gsm8[qt]])
            for qt in range(8):
                S.op("dve", lambda e: e.max(out=gsm8[:, qt, 16:24], in_=gsm8[:, qt, 0:16]), reads=[b_gsm8[qt]], writes=[b_gsm8[qt]])
            for qt in range(8):
                S.op("dve", lambda e: e.tensor_scalar(out=gsm8[:, qt, 24:25], in0=gsm8[:, qt, 18:19], scalar1=-1e29, scalar2=None, op0=ALU.max),
                     reads=[b_gsm8[qt]], writes=[b_gsm8[qt]])
            for qt in range(8):
                S.op("dve", lambda e: e.tensor_scalar(out=gsm8[:, qt, 32:48], in0=gsm8[:, qt, 0:16], scalar1=gsm8[:, qt, 24:25], scalar2=1.0,
                                                      op0=ALU.is_ge, op1=ALU.subtract),
                     reads=[b_gsm8[qt]], writes=[b_gsm8[qt]])
            for qt in range(8):
                S.op("dve", lambda e: e.tensor_scalar(out=selbp8[:, qt, 0:16], in0=gsm8[:, qt, 32:48], scalar1=-NEG, scalar2=None, op0=ALU.mult),
                     reads=[b_gsm8[qt]], writes=[b_selbp8[qt]])
            for qt in range(8):
                qs = slice(qt * 128, (qt + 1) * 128)
                tb = 4 + trc[0] % 2
                trc[0] += 1
                S.op("pe", lambda e: e.transpose(out=pbf(tb)[:, 0:128], in_=selbp8[:, qt, :], identity=ident[:, :]),
                     reads=[b_selbp8[qt], b_ident], writes=[b_pb[tb]])
                S.op("act", lambda e: e.copy(out=maskT[0:16, qs], in_=pbf(tb)[0:16, 0:128]), reads=[b_pb[tb]], writes=[b_maskT])

            cut(5)
            if h + 1 < nheads:
                load_w1(h + 1, eng="dve")
            stbanks = (0, 1, 6)
            stc = [0]
            obanks = (2, 3, 6, 7)
            qkc = [0]
            for P in range(2):
                items = []
                for kt in range(24):
                    items.append(dict(kt=kt, q0=512 * P, w=512, mask=("sel", kt // 2),
                                      pv=[(128 * a_, obanks[a_], False) for a_ in range(4)]))
                for qb in range(2):
                    i = 2 * P + qb
                    oa_, ob_ = obanks[2 * qb], obanks[2 * qb + 1]
                    for kt in range(24, 24 + 2 * i):
                        items.append(dict(kt=kt, q0=256 * i, w=256, mask=("sel", kt // 2), pv=[(0, oa_, False), (128, ob_, False)]))
                    items.append(dict(kt=24 + 2 * i, q0=256 * i, w=256, mask=("dm",), pv=[(0, oa_, True), (128, ob_, False)]))
                    items.append(dict(kt=24 + 2 * i + 1, q0=256 * i + 128, w=128, mask=("dm",), pv=[(0, ob_, True)]))
                first = {ob: True for ob in obanks}

                def emit_qk(it):
                    bk = (0, 1)[qkc[0] % 2]
                    pi = qkc[0] % 3
                    qkc[0] += 1
                    kt, q0, w = it["kt"], it["q0"], it["w"]
                    kts = slice(kt * 128, (kt + 1) * 128)
                    g_ = kt // 4
                    S.op("pe", lambda e: e.matmul(pb[bk][:, 0:w], lhsT=KT[:, kts], rhs=QT[:, q0:q0 + w], start=True, stop=False),
                         reads=[b_KT[g_], b_QT], writes=[b_pb[bk]])
                    if it["mask"][0] == "sel":
                        j = it["mask"][1]
                        S.op("pe", lambda e: e.matmul(pb[bk][:, 0:w], lhsT=selhot[:, j, :], rhs=maskT[:, q0:q0 + w], start=False, stop=True),
                             reads=[b_c1, b_maskT], writes=[b_pb[bk]])
                    else:
                        S.op("pe", lambda e: e.matmul(pb[bk][:, 0:w], lhsT=ident[:, :], rhs=dmask[:, 0:w], start=False, stop=True),
                             reads=[b_ident, b_c1], writes=[b_pb[bk]])
                    S.op("act", lambda e: e.activation(out=PT[pi][:, 0:w], in_=pb[bk][:, 0:w], func=AF.Exp, scale=SCALE),
                         reads=[b_pb[bk]], writes=[b_PT[pi]])
                    it["pi"] = pi

                def emit_pv(it):
                    kt, pi = it["kt"], it["pi"]
                    g_ = kt // 4
                    for pc, ob, last in it["pv"]:
                        S.op("pe", lambda e: e.matmul(pb[ob][:, 0:129], lhsT=PT[pi][:, pc:pc + 128], rhs=V[:, kt, :],
                                                      start=first[ob], stop=last),
                             reads=[b_PT[pi], b_V[g_]], writes=[b_pb[ob]])
                        first[ob] = False

                pend = []
                for it in items:
                    emit_qk(it)
                    pend.append(it)
                    if len(pend) > 2:
                        emit_pv(pend.pop(0))
                while pend:
                    emit_pv(pend.pop(0))
                for a_ in range(4):
                    qt = 4 * P + a_
                    ob = obanks[a_]
                    ri = qt % 2
                    S.op("dve", lambda e: e.reciprocal(out=rec[:, ri:ri + 1], in_=pb[ob][:, 128:129]),
                         reads=[b_pb[ob]], writes=[b_rec[ri]])
                    S.op("dve", lambda e: e.tensor_scalar(out=OAt[ri][:, :], in0=pb[ob][:, 0:128], scalar1=rec[:, ri:ri + 1],
                                                          scalar2=None, op0=ALU.mult),
                         reads=[b_pb[ob], b_rec[ri]], writes=[b_OAt[ri]])
                    tb = 4 + trc[0] % 2
                    trc[0] += 1
                    S.op("pe", lambda e: e.transpose(out=pbf(tb)[:, 0:128], in_=OAt[ri][:, :], identity=ident[:, :]),
                         reads=[b_OAt[ri], b_ident], writes=[b_pb[tb]])
                    S.op("act", lambda e: e.copy(out=OATs[:, qt * 128:(qt + 1) * 128], in_=pbf(tb)[:, 0:128]),
                         reads=[b_pb[tb]], writes=[b_OATs])
            S.dma("sp", "oa", oy_scr[h * 128:(h + 1) * 128, :], OATs[:, :], reads=[b_OATs], writes=[b_oy])

            cut(6)
        retA(nheads - 1)
        retB(nheads - 1)
        retC(nheads - 1)
        S.barrier()
        if upto == "p1" and debug:
            for nm, t_ in [("KT", KT), ("QT", QT), ("QRT", QRT), ("KRT", KRT), ("V", V), ("kms", kms), ("maskT", maskT),
                           ("Rst", Rst), ("Rb", Rb), ("RVo", RVo), ("SGo", SGo), ("OATs", OATs), ("YRTs", YRTs), ("W1b", W1b)]:
                shp = list(t_.shape)
                d_ = dbg_out("dbg_" + nm, shp, t_.dtype)
                sl = tuple(slice(None) for _ in shp)
                S.dma("sp", "dd_" + nm, d_[sl], t_[sl])
            S.barrier()

    except _Cut:
        return nc, dbg
    es01.close()

    if upto == "p1":
        d = dbg_out("dbg_oy", [3072, NOWN], BF16)
        with ExitStack() as es:
            t_ = es.enter_context(nc.sbuf_tensor("dbgt", [128, 24, NOWN], BF16)); b_t = Buf("dbgt")
            for kk in range(0, 24, 8):
                S.dma("sp", f"dl{kk}", t_[:, kk:kk + 8, :], oy_scr.rearrange("(k p) t -> p k t", p=128)[:, kk:kk + 8, :], reads=[b_oy], writes=[b_t])
                S.dma("sp", f"ds{kk}", d.rearrange("(k p) t -> p k t", p=128)[:, kk:kk + 8, :], t_[:, kk:kk + 8, :], reads=[b_t])
            S.barrier()
        return nc, dbg

    bufA = nc.alloc_sbuf_tensor("s_bufA", [128, 16, NOWN], BF16)
    b_xres = [Buf(f"xres{t}") for t in range(8)]
    sig_f = AF.Sigmoid
    if True:
        mixT = bufA; b_mixT = [Buf(f"mixT{i}") for i in range(2)]
        with ExitStack() as es:
            def tmp(name, shape, dt=F32):
                UNIQ[0] += 1
                return es.enter_context(nc.sbuf_tensor(f"t{UNIQ[0]}_" + name, list(shape), dt))
            hTo = tmp("hTo", [128, 16, NOWN], BF16); b_hTo = Buf("hTo")
            OAT = tmp("OAT", [128, 8, NOWN], BF16); YRT = tmp("YRT", [128, 16, NOWN], BF16); b_OY = Buf("OY")
            for g in (6, 7):
                S.dma("sp", f"ho{g}", hTo[:, :, (g - 6) * 512:(g - 5) * 512], hT_v[:, :, g * 512:(g + 1) * 512],
                      reads=[b_scr[g]], writes=[b_hTo])
            S.dma("sp", "oal", OAT[:, :, :], oy_scr[0:1024, :].rearrange("(k p) t -> p k t", p=128), reads=[b_oy], writes=[b_OY])
            for k8 in range(2):
                S.dma("sp", f"yrl{k8}", YRT[:, 8 * k8:8 * k8 + 8, :],
                      oy_scr[1024 + 1024 * k8:2048 + 1024 * k8, :].rearrange("(k p) t -> p k t", p=128), reads=[b_oy], writes=[b_OY])
            stgX = [tmp(f"stgX{i}", [128, 16, 128]) for i in range(4)]; b_stgX = [Buf(f"stgX{i}") for i in range(4)]
            sx = [0]
            Wgb = [tmp(f"Wgb{i}", [128, 16, 256], BF16) for i in range(2)]; b_Wgb = [Buf(f"Wgb{i}") for i in range(2)]
            Wmb = [tmp(f"Wmb{i}", [128, 8, 128], BF16) for i in range(2)]; b_Wmb = [Buf(f"Wmb{i}") for i in range(2)]
            Wrb = [tmp(f"Wrb{i}", [128, 16, 128], BF16) for i in range(2)]; b_Wrb = [Buf(f"Wrb{i}") for i in range(2)]
            sg = [tmp(f"sg{i}", [128, 512]) for i in range(4)]; b_sg = [Buf(f"sg{i}") for i in range(4)]
            tt = [tmp(f"tt{i}", [128, 512]) for i in range(4)]; b_tt = [Buf(f"tt{i}") for i in range(4)]

            def load_fc(fc):
                k = fc % 2

                def one(src_ap, nchunk, dst_ap, b_dst, fold):
                    i_ = sx[0] % 4
                    sx[0] += 1
                    S.dma("act", f"sx{i_}", stgX[i_][:, 0:nchunk, :], src_ap, writes=[b_stgX[i_]])
                    if fold:
                        S.op("pool", lambda e: e.tensor_tensor(out=dst_ap, in0=stgX[i_][:, 0:nchunk, :], in1=bc_last(nw1T, 16, 128), op=ALU.mult),
                             reads=[b_stgX[i_], b_nw], writes=[b_dst])
                    else:
                        S.op("pool", lambda e: e.tensor_copy(out=dst_ap, in_=stgX[i_][:, 0:nchunk, :]), reads=[b_stgX[i_]], writes=[b_dst])
                one(w_in[:, 9216 + fc * 128:9216 + (fc + 1) * 128].rearrange("(c p) n -> p c n", p=128), 16, Wgb[k][:, :, 0:128], b_Wgb[k], True)
                one(w_in[:, 11264 + fc * 128:11264 + (fc + 1) * 128].rearrange("(c p) n -> p c n", p=128), 16, Wgb[k][:, :, 128:256], b_Wgb[k], True)
                one(wbm[:, fc * 128:(fc + 1) * 128].rearrange("(c p) n -> p c n", p=128), 8, Wmb[k][:, :, :], b_Wmb[k], False)
                one(wbr[:, fc * 128:(fc + 1) * 128].rearrange("(c p) n -> p c n", p=128), 16, Wrb[k][:, :, :], b_Wrb[k], False)

            load_fc(0)
            for fc in range(16):
                k = fc % 2
                if fc + 1 < 16:
                    load_fc(fc + 1)
                for th in range(2):
                    ts_ = slice(th * 512, (th + 1) * 512)
                    st_ = (2 * fc + th) % 2
                    base = 4 * st_
                    for c in range(16):
                        S.op("pe", lambda e: e.matmul(pb[base][:, :], lhsT=Wgb[k][:, c, 0:128], rhs=hTo[:, c, ts_], start=(c == 0), stop=(c == 15)),
                             reads=[b_Wgb[k], b_hTo], writes=[b_pb[base]])
                    for c in range(16):
                        S.op("pe", lambda e: e.matmul(pb[base + 1][:, :], lhsT=Wgb[k][:, c, 128:256], rhs=hTo[:, c, ts_], start=(c == 0), stop=(c == 15)),
                             reads=[b_Wgb[k], b_hTo], writes=[b_pb[base + 1]])
                    for c in range(8):
                        S.op("pe", lambda e: e.matmul(pb[base + 2][:, :], lhsT=Wmb[k][:, c, :], rhs=OAT[:, c, ts_], start=(c == 0), stop=(c == 7)),
                             reads=[b_Wmb[k], b_OY], writes=[b_pb[base + 2]])
                    for c in range(16):
                        S.op("pe", lambda e: e.matmul(pb[base + 3][:, :], lhsT=Wrb[k][:, c, :], rhs=YRT[:, c, ts_], start=(c == 0), stop=(c == 15)),
                             reads=[b_Wrb[k], b_OY], writes=[b_pb[base + 3]])
                    i0, i1 = 2 * st_, 2 * st_ + 1
                    S.op("act", lambda e: e.activation(out=sg[i0][:, :], in_=pb[base][:, :], func=sig_f), reads=[b_pb[base]], writes=[b_sg[i0]])
                    S.op("act", lambda e: e.activation(out=sg[i1][:, :], in_=pb[base + 1][:, :], func=sig_f), reads=[b_pb[base + 1]], writes=[b_sg[i1]])
                    S.op("dve", lambda e: e.tensor_tensor(out=tt[i0][:, :], in0=pb[base + 2][:, :], in1=sg[i0][:, :], op=ALU.mult),
                         reads=[b_pb[base + 2], b_sg[i0]], writes=[b_tt[i0]])
                    S.op("dve", lambda e: e.tensor_tensor(out=tt[i1][:, :], in0=pb[base + 3][:, :], in1=sg[i1][:, :], op=ALU.mult),
                         reads=[b_pb[base + 3], b_sg[i1]], writes=[b_tt[i1]])
                    S.op("pool", lambda e: e.tensor_tensor(out=mixT[:, fc, ts_], in0=tt[i0][:, :], in1=tt[i1][:, :], op=ALU.add),
                         reads=[b_tt[i0], b_tt[i1]], writes=[b_mixT[th]])
            S.barrier()

        if upto == "p2m":
            d = dbg_out("dbg_mixT", [128, 16, NOWN], BF16)
            S.dma("sp", "dmix", d[:, :, :], mixT[:, :, :], reads=b_mixT)
            S.barrier()
            return nc, dbg

        xres = nc.alloc_sbuf_tensor("s_xres", [128, 8, D], F32)
        with ExitStack() as es:
            def tmp(name, shape, dt=F32):
                UNIQ[0] += 1
                return es.enter_context(nc.sbuf_tensor(f"t{UNIQ[0]}_" + name, list(shape), dt))
            for t in range(8):
                S.dma("sp", f"xr{t}", xres[:, t, :], xs[OWN0 + t * 128:OWN0 + (t + 1) * 128, :], writes=[b_xres[t]])
            stgW = [tmp(f"stgW{i}", [128, 8, 512]) for i in range(2)]; b_stgW = [Buf(f"stgW{i}") for i in range(2)]
            Wob = [tmp(f"Wob{i}", [128, 16, 512], BF16) for i in range(2)]; b_Wob = [Buf(f"Wob{i}") for i in range(2)]
            sc = [0]

            def load_wo(cg):
                k = cg % 2
                v = wout[:, cg * 512:(cg + 1) * 512].rearrange("(c p) n -> p c n", p=128)
                for half in range(2):
                    s_ = sc[0] % 2
                    sc[0] += 1
                    S.dma("act", f"wo{s_}", stgW[s_][:, :, :], v[:, 8 * half:8 * half + 8, :], writes=[b_stgW[s_]])
                    S.op("pool", lambda e: e.tensor_copy(out=Wob[k][:, 8 * half:8 * half + 8, :], in_=stgW[s_][:, :, :]),
                         reads=[b_stgW[s_]], writes=[b_Wob[k]])
            load_wo(0)
            bc_ = [0]
            for cg in range(4):
                k = cg % 2
                if cg + 1 < 4:
                    load_wo(cg + 1)
                for t in range(8):
                    bk = bc_[0] % 8
                    bc_[0] += 1
                    for c in range(16):
                        S.op("pe", lambda e: e.matmul(pb[bk][:, :], lhsT=mixT[:, c, t * 128:(t + 1) * 128], rhs=Wob[k][:, c, :],
                                                      start=(c == 0), stop=(c == 15)),
                             reads=[b_Wob[k]] + b_mixT, writes=[b_pb[bk]])
                    S.op("dve", lambda e: e.tensor_tensor(out=xres[:, t, cg * 512:(cg + 1) * 512], in0=pb[bk][:, :],
                                                          in1=xres[:, t, cg * 512:(cg + 1) * 512], op=ALU.add),
                         reads=[b_pb[bk], b_xres[t]], writes=[b_xres[t]])
            S.barrier()

    if upto == "p2a":
        d = dbg_out("dbg_x2", [NOWN, D])
        for t in range(8):
            S.dma("sp", f"dx{t}", d[t * 128:(t + 1) * 128, :], xres[:, t, :], reads=[b_xres[t]])
        S.barrier()
        return nc, dbg

    with ExitStack() as es:
        def tmp(name, shape, dt=F32):
            UNIQ[0] += 1
            return es.enter_context(nc.sbuf_tensor(f"t{UNIQ[0]}_" + name, list(shape), dt))
        h2T = bufA; b_h2T = Buf("h2T"); b_h2Tp = [Buf(f"h2Tp{i}") for i in range(32)]
        Wring = [tmp(f"Wring{i}", [128, 8192], BF16) for i in range(3)]; b_Wring = [Buf(f"Wring{i}") for i in range(3)]
        stgE = [tmp(f"stgE{i}", [128, 2048]) for i in range(3)]; b_stgE = [Buf(f"stgE{i}") for i in range(3)]
        actT = tmp("actT", [128, 4, NOWN], BF16); b_actT = [Buf(f"actT{i}") for i in range(2)]
        silt = [tmp(f"silt{i}", [128, 512]) for i in range(2)]; b_silt = [Buf(f"silt{i}") for i in range(2)]
        comb = tmp("comb", [128, 8, 32]); b_combs = [Buf(f"comb{t}") for t in range(8)]
        rs = tmp("rs", [128, 64]); b_rs = [Buf(f"rs{i}") for i in range(2)]
        wr36s = tmp("wr36s", [128, 16, 36]); wr36b = tmp("wr36b", [128, 16, 36], BF16); b_wr36 = Buf("wr36")
        brB = tmp("brB", [128, 36]); b_brB = Buf("brB")
        S.dma("sp", "wr36", wr36s[:, :, :], wr36_d.rearrange("(c p) n -> p c n", p=128), writes=[b_wr36])
        S.dma("sp", "brB", brB[:, :], bass.AP(br36_d, 0, [[0, 128], [1, 36]]), writes=[b_brB])
        S.op("pool", lambda e: e.tensor_tensor(out=wr36b[:, :, :], in0=wr36s[:, :, :], in1=bc_last(nw2T, 16, 36), op=ALU.mult),
             reads=[b_wr36, b_nw], writes=[b_wr36])

        wc = [0]; sce = [0]

        def load_mat(kind, e_):
            k = wc[0] % 3
            wc[0] += 1
            if kind in ("g", "u"):
                v = (weg if kind == "g" else weu)[e_].rearrange("(c p) n -> p c n", p=128)
                wv = Wring[k][:, :].rearrange("p (c n) -> p c n", n=512)
                for q4 in range(4):
                    s_ = sce[0] % 3
                    sce[0] += 1
                    sv = stgE[s_][:, :].rearrange("p (c n) -> p c n", n=512)
                    S.dma("sp", f"se{s_}", sv, v[:, 4 * q4:4 * q4 + 4, :], writes=[b_stgE[s_]])
                    if kind == "g":
                        S.op("pool", lambda e: e.tensor_tensor(out=wv[:, 4 * q4:4 * q4 + 4, :], in0=sv,
                                                               in1=bc_last(nw2T, 4, 512, off=4 * q4), op=ALU.mult),
                             reads=[b_stgE[s_], b_nw], writes=[b_Wring[k]])
                    else:
                        for c in range(4):
                            cc = 4 * q4 + c
                            S.op("act", lambda e: e.activation(out=wv[:, cc, :], in_=sv[:, c, :], func=AF.Copy, scale=nw2T[:, cc:cc + 1]),
                                 reads=[b_stgE[s_], b_nw], writes=[b_Wring[k]])
            else:
                v = wed[e_].rearrange("(f p) n -> p f n", p=128)
                wv = Wring[k][:, :].rearrange("p (f n) -> p f n", n=2048)
                for q4 in range(4):
                    s_ = sce[0] % 3
                    sce[0] += 1
                    S.dma("sp", f"se{s_}", stgE[s_][:, :], v[:, q4, :], writes=[b_stgE[s_]])
                    S.op("dve", lambda e: e.tensor_copy(out=wv[:, q4, :], in_=stgE[s_][:, :]),
                         reads=[b_stgE[s_]], writes=[b_Wring[k]])
            return k

        kg = load_mat("g", 0)
        st = tmp("st2", [128, 8]); b_st = [Buf(f"st2_{i}") for i in range(4)]
        with ExitStack() as es3:
            def tmp3(name, shape, dt=F32):
                UNIQ[0] += 1
                return es3.enter_context(nc.sbuf_tensor(f"t{UNIQ[0]}_" + name, list(shape), dt))
            junk = tmp3("junk2", [128, D], BF16); b_junk = Buf("junk2")
            hb = [tmp3(f"hb2_{i}", [128, D], BF16) for i in range(2)]; b_hb = [Buf(f"hb2_{i}") for i in range(2)]

            def n2_a(t):
                j = t % 4
                hk = t % 2
                rms_tile(xres[:, t, :], b_xres[t], junk, b_junk, st, b_st[j], j)
                S.op("dve", lambda e: e.tensor_scalar(out=hb[hk][:, :], in0=xres[:, t, :], scalar1=st[:, 2 * j + 1:2 * j + 2],
                                                      scalar2=None, op0=ALU.mult),
                     reads=[b_xres[t], b_st[j]], writes=[b_hb[hk]])

            def n2_b(t):
                hk = t % 2
                transpose_tile(hb[hk], b_hb[hk], lambda cq: h2T[:, 4 * cq:4 * cq + 4, t * 128:(t + 1) * 128],
                               lambda cq: b_h2Tp[t * 4 + cq])
            n2_a(0)
            for t in range(8):
                if t + 1 < 8:
                    n2_a(t + 1)
                n2_b(t)
            ku = load_mat("u", 0)
            S.barrier()

        Ls = tmp("Ls", [128, 8, 36]); b_L = [Buf(f"L{t}") for t in range(8)]
        rs8 = tmp("rs8", [128, 8, 32]); b_rs8 = [Buf(f"rs8_{t}") for t in range(8)]
        rbanks = (6, 7, 4, 5)
        T8 = range(8)
        for t in T8:
            rb_ = rbanks[t % 4]
            for c in range(16):
                S.op("pe", lambda e: e.matmul(pb[rb_][:, 0:36], lhsT=h2T[:, c, t * 128:(t + 1) * 128], rhs=wr36b[:, c, :],
                                              start=(c == 0), stop=(c == 15)),
                     reads=[b_h2T, b_wr36], writes=[b_pb[rb_]])
            S.op("dve", lambda e: e.tensor_tensor(out=Ls[:, t, :], in0=pb[rb_][:, 0:36], in1=brB[:, :], op=ALU.add),
                 reads=[b_pb[rb_], b_brB], writes=[b_L[t]])

        def rstage(fn, rd_L=True, wr_L=False, eng="dve"):
            for t in T8:
                rds = [b_rs8[t]] + ([b_L[t]] if rd_L else [])
                wrs = [b_L[t]] if wr_L else [b_rs8[t]]
                S.op(eng, (lambda t_: (lambda e: fn(e, t_)))(t), reads=rds, writes=wrs)
        rstage(lambda e, t: e.tensor_reduce(out=rs8[:, t, 0:1], in_=Ls[:, t, 0:4], axis=mybir.AxisListType.X, op=ALU.max))
        rstage(lambda e, t: e.tensor_scalar(out=rs8[:, t, 1:2], in0=rs8[:, t, 0:1], scalar1=-1.0, scalar2=None, op0=ALU.mult))
        rstage(lambda e, t: e.tensor_scalar(out=rs8[:, t, 4:8], in0=Ls[:, t, 0:4], scalar1=rs8[:, t, 0:1], scalar2=None, op0=ALU.is_equal))
        rstage(lambda e, t: e.activation(out=rs8[:, t, 8:12], in_=Ls[:, t, 0:4], func=AF.Exp, bias=rs8[:, t, 1:2],
                                         accum_out=rs8[:, t, 2:3]), eng="act")
        rstage(lambda e, t: e.reciprocal(out=rs8[:, t, 3:4], in_=rs8[:, t, 2:3]))
        rstage(lambda e, t: e.tensor_scalar(out=rs8[:, t, 12:20], in0=Ls[:, t, 4:12], scalar1=rs8[:, t, 4:5], scalar2=None, op0=ALU.mult))
        for g in range(1, 4):
            rstage(lambda e, t, g=g: e.scalar_tensor_tensor(out=rs8[:, t, 12:20], in0=Ls[:, t, 4 + 8 * g:12 + 8 * g],
                                                            scalar=rs8[:, t, 4 + g:5 + g], in1=rs8[:, t, 12:20],
                                                            op0=ALU.mult, op1=ALU.add))
        rstage(lambda e, t: e.max(out=rs8[:, t, 20:28], in_=rs8[:, t, 12:20]))
        rstage(lambda e, t: e.tensor_tensor(out=rs8[:, t, 28:29], in0=rs8[:, t, 20:21], in1=rs8[:, t, 21:22], op=ALU.subtract))
        rstage(lambda e, t: e.activation(out=rs8[:, t, 29:30], in_=rs8[:, t, 28:29], func=AF.Exp, scale=-1.0), eng="act")
        rstage(lambda e, t: e.tensor_scalar(out=rs8[:, t, 29:30], in0=rs8[:, t, 29:30], scalar1=1.0, scalar2=None, op0=ALU.add))
        rstage(lambda e, t: e.reciprocal(out=rs8[:, t, 29:30], in_=rs8[:, t, 29:30]))
        rstage(lambda e, t: e.tensor_scalar(out=rs8[:, t, 30:31], in0=rs8[:, t, 29:30], scalar1=-1.0, scalar2=1.0,
                                            op0=ALU.mult, op1=ALU.add))
        rstage(lambda e, t: e.tensor_scalar(out=Ls[:, t, 4:12], in0=rs8[:, t, 12:20], scalar1=rs8[:, t, 20:21],
                                            scalar2=rs8[:, t, 29:30], op0=ALU.is_equal, op1=ALU.mult), wr_L=True)
        rstage(lambda e, t: e.tensor_scalar(out=Ls[:, t, 12:20], in0=rs8[:, t, 12:20], scalar1=rs8[:, t, 21:22],
                                            scalar2=rs8[:, t, 30:31], op0=ALU.is_equal, op1=ALU.mult), wr_L=True)
        rstage(lambda e, t: e.tensor_tensor(out=Ls[:, t, 4:12], in0=Ls[:, t, 4:12], in1=Ls[:, t, 12:20], op=ALU.add), wr_L=True)
        rstage(lambda e, t: e.tensor_scalar(out=rs8[:, t, 8:12], in0=rs8[:, t, 4:8], scalar1=rs8[:, t, 3:4], scalar2=None, op0=ALU.mult))
        for g in range(4):
            for t in T8:
                S.op("dve", lambda e: e.tensor_scalar(out=comb[:, t, 8 * g:8 * g + 8], in0=Ls[:, t, 4:12], scalar1=rs8[:, t, 8 + g:9 + g],
                                                      scalar2=None, op0=ALU.mult),
                     reads=[b_L[t], b_rs8[t]], writes=[b_combs[t]])

        if upto == "p2r":
            d = dbg_out("dbg_comb", [128, 8, 32])
            S.dma("sp", "dcomb", d[:, :, :], comb[:, :, :], reads=b_combs)
            S.barrier()
            return nc, dbg

        kd = load_mat("d", 0)
        gc = [0]; yc = [0]
        for e_ in range(NEXP):
            wg_v = Wring[kg][:, :].rearrange("p (c n) -> p c n", n=512)
            wu_v = Wring[ku][:, :].rearrange("p (c n) -> p c n", n=512)
            for th in range(2):
                ts_ = slice(th * 512, (th + 1) * 512)
                for fq in range(4):
                    pr = gc[0] % 2
                    gc[0] += 1
                    bg, bu = 2 * pr, 2 * pr + 1
                    for c in range(16):
                        S.op("pe", lambda e: e.matmul(pb[bg][:, :], lhsT=wg_v[:, c, fq * 128:(fq + 1) * 128], rhs=h2T[:, c, ts_],
                                                      start=(c == 0), stop=(c == 15)),
                             reads=[b_Wring[kg], b_h2T], writes=[b_pb[bg]])
                    for c in range(16):
                        S.op("pe", lambda e: e.matmul(pb[bu][:, :], lhsT=wu_v[:, c, fq * 128:(fq + 1) * 128], rhs=h2T[:, c, ts_],
                                                      start=(c == 0), stop=(c == 15)),
                             reads=[b_Wring[ku], b_h2T], writes=[b_pb[bu]])
                    S.op("act", lambda e: e.activation(out=silt[pr][:, :], in_=pb[bg][:, :], func=AF.Silu),
                         reads=[b_pb[bg]], writes=[b_silt[pr]])
                    S.op("dve", lambda e: e.tensor_tensor(out=actT[:, fq, ts_], in0=pb[bu][:, :], in1=silt[pr][:, :], op=ALU.mult),
                         reads=[b_pb[bu], b_silt[pr]], writes=[b_actT[th]])
            if e_ + 1 < NEXP:
                kg_n = load_mat("g", e_ + 1)
                ku_n = load_mat("u", e_ + 1)
            wd_v = Wring[kd][:, :].rearrange("p (f n) -> p f n", n=2048)
            for t in range(8):
                for cg in range(4):
                    bk = 4 + yc[0] % 4
                    yc[0] += 1
                    for fq in range(4):
                        S.op("pe", lambda e: e.matmul(pb[bk][:, :], lhsT=actT[:, fq, t * 128:(t + 1) * 128],
                                                      rhs=wd_v[:, fq, cg * 512:(cg + 1) * 512], start=(fq == 0), stop=(fq == 3)),
                             reads=[b_Wring[kd], b_actT[t // 4]], writes=[b_pb[bk]])
                    S.op("dve", lambda e: e.scalar_tensor_tensor(out=xres[:, t, cg * 512:(cg + 1) * 512], in0=pb[bk][:, :],
                                                                 scalar=comb[:, t, e_:e_ + 1], in1=xres[:, t, cg * 512:(cg + 1) * 512],
                                                                 op0=ALU.mult, op1=ALU.add),
                         reads=[b_pb[bk], b_combs[t], b_xres[t]], writes=[b_xres[t]])
            if e_ + 1 < NEXP:
                kd = load_mat("d", e_ + 1)
                kg, ku = kg_n, ku_n
        S.barrier()

    with ExitStack() as es:
        def tmp(name, shape, dt=F32):
            UNIQ[0] += 1
            return es.enter_context(nc.sbuf_tensor(f"t{UNIQ[0]}_" + name, list(shape), dt))
        junk = tmp("junk3", [128, D], BF16); b_junk = Buf("junk3")
        nwfB = tmp("nwfB", [128, D]); b_nwf = Buf("nwf")
        S.dma("sp", "nwf", nwfB[:, :], bass.AP(nwf_d, 0, [[0, 128], [1, D]]), writes=[b_nwf])
        ot = [tmp(f"ot{i}", [128, D]) for i in range(2)]; b_ot = [Buf(f"ot{i}") for i in range(2)]
        st = tmp("st3", [128, 8]); b_st = [Buf(f"st3_{i}") for i in range(4)]
        toks = []
        for t in range(8):
            j = t % 4
            rms_tile(xres[:, t, :], b_xres[t], junk, b_junk, st, b_st[j], j)
            k = t % 2
            S.op("dve", lambda e: e.scalar_tensor_tensor(out=ot[k][:, :], in0=xres[:, t, :], scalar=st[:, 2 * j + 1:2 * j + 2],
                                                         in1=nwfB[:, :], op0=ALU.mult, op1=ALU.mult),
                 reads=[b_xres[t], b_st[j], b_nwf], writes=[b_ot[k]])
            toks.append(S.dma("sp", f"out{k}", out[t * 128:(t + 1) * 128, :], ot[k][:, :], reads=[b_ot[k]]))
        S.barrier()
    return nc, dbg


def host_consts(qi):
    f32 = np.float32
    bf = ml_dtypes.bfloat16
    c = {}
    c["ident"] = np.eye(128, dtype=f32).astype(bf)
    kk = np.arange(128)[:, None]; qq = np.arange(128)[None, :]
    causal = np.where(kk <= qq, 0.0, NEG).astype(f32)
    c["dmask"] = np.concatenate([causal, np.zeros((128, 128), f32)], axis=1).astype(bf)
    sh = np.zeros((128, 16, 128), f32)
    for j in range(16):
        sh[j, j, :] = 1.0
    c["selhot"] = sh.astype(bf)
    pm = np.full((4, 16), -1e30, f32)
    for i in range(4):
        pm[i, 4 * (3 - qi):12 + i] = 0.0
    c["pmask"] = np.ascontiguousarray(np.broadcast_to(pm[None], (128, 4, 16)))
    half = 64
    inv = (np.float32(10000.0) ** (-(np.arange(half, dtype=f32) / np.float32(half)))).astype(f32)
    pos = (np.arange(NSLOT) - (3 - qi) * 1024).astype(f32)
    ang = (pos[:, None] * inv[None, :]).astype(f32)
    cs, sn = np.cos(ang).astype(f32).T, np.sin(ang).astype(f32).T
    rot = np.zeros((128, 2, NSLOT), f32)
    rot[0:64, 0] = cs; rot[64:128, 0] = cs
    rot[0:64, 1] = -sn; rot[64:128, 1] = sn
    c["rot"] = rot
    hh = np.arange(NH, dtype=f32)
    lg = np.log(f32(1.0) - f32(2.0) ** (f32(-5.0) - hh)).astype(f32)
    idx = np.arange(128, dtype=f32)
    diff = idx[None, :] - idx[:, None]
    dk = f32(128.0 ** -0.5)
    dT = np.zeros((128, NH, 128), f32)
    for h in range(NH):
        dT[:, h, :] = np.where(diff >= 0, np.exp(lg[h] * np.maximum(diff, 0.0)), 0.0) * dk
    c["dT"] = dT
    cols = np.zeros((128, 3, NH), f32)
    for h in range(NH):
        cols[:, 0, h] = np.exp(lg[h] * (idx + 1.0))
        cols[:, 1, h] = np.exp(lg[h] * (127.0 - idx)) * dk
        cols[:, 2, h] = np.exp(lg[h] * 128.0)
    c["cols"] = cols
    return c


def make_in_maps(x, norm_mix_w, w_in, ret_gn_w, w_branch_moba, w_branch_ret, w_out, norm_ffn_w,
                 w_router_group, b_router_group, w_router_expert, b_router_expert,
                 w_expert_gate, w_expert_up, w_expert_down, norm_final_w):
    f32 = np.float32
    x = np.asarray(x, f32)
    shared = {
        "w_in": np.ascontiguousarray(np.asarray(w_in, f32)[0]),
        "nw1T": np.ascontiguousarray(np.asarray(norm_mix_w, f32)[0].reshape(16, 128).T),
        "nw2T": np.ascontiguousarray(np.asarray(norm_ffn_w, f32)[0].reshape(16, 128).T),
        "nwf": np.asarray(norm_final_w, f32).reshape(1, D),
        "gnw": np.asarray(ret_gn_w, f32)[0].reshape(1, D),
        "wbm": np.ascontiguousarray(np.asarray(w_branch_moba, f32)[0]),
        "wbr": np.ascontiguousarray(np.asarray(w_branch_ret, f32)[0]),
        "wout": np.ascontiguousarray(np.asarray(w_out, f32)[0]),
        "wr36": np.ascontiguousarray(np.concatenate(
            [np.asarray(w_router_group, f32)[0]] + [np.asarray(w_router_expert, f32)[0, g] for g in range(4)], axis=1)),
        "br36": np.concatenate([np.asarray(b_router_group, f32)[0].reshape(-1),
                                np.asarray(b_router_expert, f32)[0].reshape(-1)]).reshape(1, 36),
        "weg": np.ascontiguousarray(np.asarray(w_expert_gate, f32)[0]),
        "weu": np.ascontiguousarray(np.asarray(w_expert_up, f32)[0]),
        "wed": np.ascontiguousarray(np.asarray(w_expert_down, f32)[0]),
    }
    consts = [host_consts(qi) for qi in range(4)]
    in_maps = []
    for c in range(8):
        b, qi = divmod(c, 4)
        xsl = np.zeros((NSLOT, D), f32)
        n = (qi + 1) * 1024
        xsl[NSLOT - n:] = x[b, :n]
        m = dict(shared)
        m.update(consts[qi])
        m["xs"] = xsl
        in_maps.append(m)
    return in_maps


_NC_CACHE = {}


def kernel(**inputs):
    in_maps = make_in_maps(**inputs)
    if "full" not in _NC_CACHE:
        _NC_CACHE["full"] = build("full")[0]
    nc = _NC_CACHE["full"]
    res = run_bass_kernel_spmd(nc, in_maps, core_ids=list(range(8)))
    outp = np.zeros((2, 4096, D), np.float32)
    for c in range(8):
        b, qi = divmod(c, 4)
        outp[b, qi * 1024:(qi + 1) * 1024] = res.results[c]["out"]
    return outp
```

```python
from contextlib import ExitStack

import numpy as np
import ml_dtypes
import concourse.bass as bass
import concourse.mybir as mybir
from concourse.bass_utils import run_bass_kernel_spmd

F32 = mybir.dt.float32
BF16 = mybir.dt.bfloat16
AF = mybir.ActivationFunctionType
ALU = mybir.AluOpType

D = 2048
NSLOT = 4096
NOWN = 1024
OWN0 = NSLOT - NOWN
NH = 8
NEXP = 32
DEXP = 512
SCALE = 128.0 ** -0.5
NEG = -30000.0
RMS_EPS = 1e-6
GN_EPS = 1e-6


DECL = []
UNIQ = [0]


class _Cut(Exception):
    pass


CUT = [0]


class Buf:
    __slots__ = ("name", "w", "r", "excl")

    def __init__(self, name, excl=False):
        self.name = name
        self.w = None
        self.r = {}
        self.excl = excl


class Sched:
    ENG = ("pe", "dve", "act", "pool", "sp")

    def __init__(self, nc):
        self.nc = nc
        self.eng = {"pe": nc.tensor, "dve": nc.vector, "act": nc.scalar,
                    "pool": nc.gpsimd, "sp": nc.sync}
        self.sem = {e: nc.alloc_semaphore("s_" + e) for e in self.ENG}
        self.cnt = {e: 0 for e in self.ENG}
        self.waited = {}
        self.dsem = {}
        self.dcnt = {}
        self.ninst = 0

    def _wait(self, x, tok):
        if tok is None:
            return
        if tok[0] == "dma":
            _, slot, val = tok
            key = (x, "dma", slot)
            if self.waited.get(key, 0) >= val:
                return
            self.eng[x].wait_ge(self.dsem[slot], val)
            self.waited[key] = val
        else:
            e, val = tok
            if e == x and x == "pe":
                return
            key = (x, e)
            if self.waited.get(key, 0) >= val:
                return
            self.eng[x].wait_ge(self.sem[e], val)
            self.waited[key] = val

    def _deps(self, x, reads, writes):
        for b in reads:
            self._wait(x, b.w)
            if b.excl:
                for t in b.r.values():
                    if t[0] != x:
                        self._wait(x, t)
        for b in writes:
            self._wait(x, b.w)
            for t in b.r.values():
                self._wait(x, t)

    @staticmethod
    def _commit(tok, reads, writes):
        k = tok[:-1]
        for b in reads:
            b.r[k] = tok
        for b in writes:
            b.w = tok
            b.r = {}

    def op(self, x, fn, reads=(), writes=()):
        self._deps(x, reads, writes)
        ins = fn(self.eng[x])
        self.cnt[x] += 1
        ins.then_inc(self.sem[x], 1)
        tok = (x, self.cnt[x])
        self._commit(tok, reads, writes)
        self.ninst += 1
        return tok

    def dma(self, q, slot, out, in_, reads=(), writes=()):
        if slot not in self.dsem:
            self.dsem[slot] = self.nc.alloc_semaphore("d_" + slot)
            self.dcnt[slot] = 0
        self._deps(q, reads, writes)
        ins = self.eng[q].dma_start(out=out, in_=in_)
        self.dcnt[slot] += 16
        ins.then_inc(self.dsem[slot], 16)
        tok = ("dma", slot, self.dcnt[slot])
        self._commit(tok, reads, writes)
        self.ninst += 1
        return tok

    def barrier(self):
        for x in self.ENG:
            for e in self.ENG:
                if e != x and self.cnt[e] > 0:
                    self._wait(x, (e, self.cnt[e]))
            for slot, v in self.dcnt.items():
                self._wait(x, ("dma", slot, v))


def build(upto="full", debug=False, nheads=NH):
    nc = bass.Bass("TRN2", target_bir_lowering=False)
    S = Sched(nc)

    DECL.clear()
    early = upto in ("p0", "p1")

    def din(name, shape, dt=F32):
        DECL.append(name)
        return nc.dram_tensor(name, list(shape), dt, kind="ExternalInput")

    xs = din("xs", [NSLOT, D]).ap()
    w_in = din("w_in", [D, 13312]).ap()
    nw1T_d = din("nw1T", [128, 16]).ap()
    nw2T_d = din("nw2T", [128, 16]).ap()
    nwf_d = din("nwf", [1, D])
    gnw_d = din("gnw", [1, D])
    rot_d = din("rot", [128, 2, NSLOT]).ap()
    dT_d = din("dT", [128, NH, 128]).ap()
    cols_d = din("cols", [128, 3, NH]).ap()
    pmask_d = din("pmask", [128, 4, 16]).ap()
    selhot_d = din("selhot", [128, 16, 128], BF16).ap()
    ident_d = din("ident", [128, 128], BF16).ap()
    dmask_d = din("dmask", [128, 256], BF16).ap()
    if not early:
        wbm = din("wbm", [1024, D]).ap()
        wbr = din("wbr", [D, D]).ap()
        wout = din("wout", [D, D]).ap()
        wr36_d = din("wr36", [D, 36]).ap()
        br36_d = din("br36", [1, 36])
        weg = din("weg", [NEXP, D, DEXP]).ap()
        weu = din("weu", [NEXP, D, DEXP]).ap()
        wed = din("wed", [NEXP, DEXP, D]).ap()
        out = nc.dram_tensor("out", [NOWN, D], F32, kind="ExternalOutput").ap()

    hT_scr = nc.dram_tensor("hT_scr", [D, NSLOT], BF16).ap()
    oy_scr = nc.dram_tensor("oy_scr", [3072, NOWN], BF16).ap()
    hT_v = hT_scr.rearrange("(c p) s -> p c s", p=128)
    b_scr = [Buf(f"scr{g}") for g in range(8)]
    b_oy = Buf("oy")

    dbg = {}

    def cut(k):
        if CUT[0] == k:
            S.barrier()
            dc = dbg_out('dbg_cut', [128, 16])
            S.dma('sp', 'dcut', dc[:, :], nw1T[:, :])
            S.barrier()
            raise _Cut()

    def dbg_out(name, shape, dt=F32):
        t = nc.dram_tensor(name, list(shape), dt, kind="ExternalOutput").ap()
        dbg[name] = t
        return t

    pb = [nc.alloc_psum_tensor(f"pb{i}", [128, 512], F32) for i in range(8)]
    b_pb = [Buf(f"pb{i}", excl=True) for i in range(8)]

    def pbf(i):
        return pb[i][:, :].bitcast(BF16)

    def sb(name, shape, dt=F32):
        return nc.alloc_sbuf_tensor("s_" + name, list(shape), dt)

    ident = sb("ident", [128, 128], BF16); b_ident = Buf("ident")
    S.dma("sp", "c0_1", ident[:, :], ident_d[:, :], writes=[b_ident])
    nw1T = sb("nw1T", [128, 16]); nw2T = sb("nw2T", [128, 16]); b_nw = Buf("nw")
    S.dma("sp", "c0_2", nw1T[:, :], nw1T_d[:, :], writes=[b_nw])
    S.dma("sp", "c0_3", nw2T[:, :], nw2T_d[:, :], writes=[b_nw])

    def bc_last(t, n_mid, w, off=0):
        return bass.AP(t, off, [[t.shape[1], 128], [1, n_mid], [0, w]])

    def rms_tile(src_ap, b_src, junk, b_junk, st, b_st, j):
        S.op("act", lambda e: e.activation(out=junk[:, :], in_=src_ap, func=AF.Square,
                                           accum_out=st[:, 2 * j:2 * j + 1]),
             reads=[b_src], writes=[b_junk, b_st])
        S.op("act", lambda e: e.activation(out=st[:, 2 * j:2 * j + 1], in_=st[:, 2 * j:2 * j + 1],
                                           func=AF.Sqrt, scale=1.0 / D, bias=RMS_EPS),
             reads=[b_st], writes=[b_st])
        S.op("dve", lambda e: e.reciprocal(out=st[:, 2 * j + 1:2 * j + 2], in_=st[:, 2 * j:2 * j + 1]),
             reads=[b_st], writes=[b_st])

    tp_cnt = [0]

    def transpose_tile(hb, b_hb, dst_fn, b_dst_fn, banks=(0, 1, 2, 3, 4, 5, 6, 7)):
        for cq in range(4):
            bk = banks[tp_cnt[0] % len(banks)]
            tp_cnt[0] += 1
            tp = pbf(bk)[:, 0:512].rearrange("p (a b) -> p a b", b=128)
            for ci in range(4):
                c = cq * 4 + ci
                S.op("pe", lambda e: e.transpose(out=tp[:, ci, :], in_=hb[:, c * 128:(c + 1) * 128],
                                                 identity=ident[:, :]),
                     reads=[b_hb, b_ident], writes=[b_pb[bk]])
            if cq % 2 == 0:
                S.op("act", lambda e: e.copy(out=dst_fn(cq), in_=tp), reads=[b_pb[bk]], writes=[b_dst_fn(cq)])
            else:
                S.op("dve", lambda e: e.tensor_copy(out=dst_fn(cq), in_=tp), reads=[b_pb[bk]], writes=[b_dst_fn(cq)])

    es01 = ExitStack()

    def tmp01(name, shape, dt=F32):
        UNIQ[0] += 1
        return es01.enter_context(nc.sbuf_tensor(f"t{UNIQ[0]}_" + name, list(shape), dt))
    W1b = tmp01("W1b", [128, 16, 1152], BF16); b_W1b = Buf("W1b")
    stg = [tmp01(f"stg{i}", [128, 16, 128]) for i in range(3)]; b_stg = [Buf(f"stg{i}") for i in range(3)]
    colmap = [(0, 0), (1024, 128), (3072, 256), (4096, 384), (2048, 512)]

    def load_w1(h, eng="pool"):
        blocks = [(base + h * 128, off) for base, off in colmap]
        blocks += [(5120 + h * 256, 640), (5120 + h * 256 + 128, 768), (7168 + h * 256, 896), (7168 + h * 256 + 128, 1024)]
        for bi, (c0, off) in enumerate(blocks):
            k = (h * 9 + bi) % 3
            S.dma("sp" if h == 0 else "pool", f"ws{k}" if h == 0 else f"wp{k}", stg[k][:, :, :], w_in[:, c0:c0 + 128].rearrange("(c p) n -> p c n", p=128),
                  writes=[b_stg[k]])
            S.op(eng, lambda e: e.tensor_tensor(out=W1b[:, :, off:off + 128], in0=stg[k][:, :, :],
                                                in1=bc_last(nw1T, 16, 128), op=ALU.mult),
                 reads=[b_stg[k], b_nw], writes=[b_W1b])

    load_w1(0)
    with ExitStack() as es:
        def tmp(name, shape, dt=F32):
            UNIQ[0] += 1
            return es.enter_context(nc.sbuf_tensor(f"t{UNIQ[0]}_" + name, list(shape), dt))
        xst = [tmp(f"xst{i}", [128, D]) for i in range(3)]; b_xst = [Buf(f"xst{i}") for i in range(3)]
        junk = tmp("junk0", [128, D], BF16); b_junk = Buf("junk")
        hb = [tmp(f"hb{i}", [128, D], BF16) for i in range(2)]; b_hb = [Buf(f"hb{i}") for i in range(2)]
        hTg = [tmp(f"hTg{i}", [128, 16, 512], BF16) for i in range(2)]
        b_hTgp = [[Buf(f"hTg{i}_{j}") for j in range(16)] for i in range(2)]
        st = tmp("st0", [128, 8]); b_st = [Buf(f"st{i}") for i in range(4)]

        def p0_a(t):
            k = t % 3
            j = t % 4
            hk = t % 2
            S.dma("sp", f"x{k}", xst[k][:, :], xs[t * 128:(t + 1) * 128, :], writes=[b_xst[k]])
            rms_tile(xst[k][:, :], b_xst[k], junk, b_junk, st, b_st[j], j)
            S.op("dve", lambda e: e.tensor_scalar(out=hb[hk][:, :], in0=xst[k][:, :],
                                                  scalar1=st[:, 2 * j + 1:2 * j + 2], scalar2=None, op0=ALU.mult),
                 reads=[b_xst[k], b_st[j]], writes=[b_hb[hk]])

        def p0_b(t):
            g, tl = divmod(t, 4)
            hk = t % 2
            gi = g % 2
            transpose_tile(hb[hk], b_hb[hk],
                           lambda cq: hTg[gi][:, 4 * cq:4 * cq + 4, tl * 128:(tl + 1) * 128],
                           lambda cq: b_hTgp[gi][tl * 4 + cq])
            if tl == 3:
                S.dma("pool", f"hs{gi}", hT_v[:, :, g * 512:(g + 1) * 512], hTg[gi][:, :, :],
                      reads=b_hTgp[gi], writes=[b_scr[g]])
        NT0 = NSLOT // 128
        p0_a(0)
        for t in range(NT0):
            if t + 1 < NT0:
                p0_a(t + 1)
            p0_b(t)
        S.barrier()

    if upto == "p0":
        d = dbg_out("dbg_hT", [D, NSLOT], BF16)
        with ExitStack() as es:
            t_ = es.enter_context(nc.sbuf_tensor("dbgt", [128, 16, 512], BF16)); b_t = Buf("dbgt")
            for g in range(8):
                S.dma("sp", "dl", t_[:, :, :], hT_v[:, :, g * 512:(g + 1) * 512], reads=[b_scr[g]], writes=[b_t])
                S.dma("sp", "ds", d.rearrange("(c p) s -> p c s", p=128)[:, :, g * 512:(g + 1) * 512], t_[:, :, :], reads=[b_t])
            S.barrier()
        return nc, dbg

    try:
      with ExitStack() as es:
        def tmp(name, shape, dt=F32):
            UNIQ[0] += 1
            return es.enter_context(nc.sbuf_tensor(f"t{UNIQ[0]}_" + name, list(shape), dt))
        rotc = None
        dT = tmp("dT", [128, NH, 128]); cols = tmp("cols", [128, 3, NH]); pmask = tmp("pmask", [128, 4, 16])
        selhot = tmp("selhot", [128, 16, 128], BF16); dmask = tmp("dmask", [128, 256], BF16)
        gnwB = tmp("gnwB", [128, D])
        b_c1 = Buf("c1")
        S.dma("sp", "c1_4", dT[:, :, :], dT_d[:, :, :], writes=[b_c1])
        S.dma("sp", "c1_5", cols[:, :, :], cols_d[:, :, :], writes=[b_c1])
        S.dma("sp", "c1_6", pmask[:, :, :], pmask_d[:, :, :], writes=[b_c1])
        S.dma("sp", "c1_7", selhot[:, :, :], selhot_d[:, :, :], writes=[b_c1])
        S.dma("sp", "c1_8", dmask[:, :], dmask_d[:, :], writes=[b_c1])
        S.dma("sp", "c1_9", gnwB[:, :], bass.AP(gnw_d, 0, [[0, 128], [1, D]]), writes=[b_c1])

        hTg = [tmp(f"hTg{i}", [128, 16, 512], BF16) for i in range(2)]; b_hTg = [Buf(f"hTg{i}") for i in range(2)]
        rt = [tmp(f"rt{i}", [128, 2, 512]) for i in range(2)]; b_rt = [Buf(f"rt{i}") for i in range(2)]
        KT = tmp("KT", [128, NSLOT], BF16); b_KT = [Buf(f"KT{g}") for g in range(8)]
        V = tmp("V", [128, 32, 129], BF16); b_V = [Buf(f"V{g}") for g in range(8)]
        QT = tmp("QT", [128, NOWN], BF16); b_QT = Buf("QT")
        QRT = tmp("QRT", [128, NOWN], BF16); b_QRT = Buf("QRT")
        KRT = tmp("KRT", [128, NOWN], BF16); b_KRT = Buf("KRT")
        KRg = [tmp(f"KRg{i}", [128, 512], BF16) for i in range(2)]; b_KRg = [Buf(f"KRg{i}") for i in range(2)]
        KZ = [tmp(f"KZ{i}", [128, 4, 128], BF16) for i in range(2)]; b_KZ = [Buf(f"KZ{i}") for i in range(2)]
        RVt = [tmp(f"RVt{i}", [128, 256], BF16) for i in range(8)]; b_RVt = [Buf(f"RVt{i}") for i in range(8)]
        RVo = tmp("RVo", [128, 8, 256], BF16); b_RVo = [Buf(f"RVo{i}") for i in range(8)]
        SGo = tmp("SGo", [128, 8, 256], BF16); b_SGo = [Buf(f"SGo{i}") for i in range(8)]
        sil = [tmp(f"sil{i}", [128, 256]) for i in range(2)]; b_sil = [Buf(f"sil{i}") for i in range(2)]
        Rst = tmp("Rst", [128, 256]); b_R = Buf("R")
        Rb = tmp("Rb", [128, 8, 256], BF16); b_Rb = [Buf(f"Rb{i}") for i in range(8)]
        kms = tmp("kms", [128, 16]); b_kms = Buf("kms")
        kmb = tmp("kmb", [128, 16], BF16); b_kmb = Buf("kmb")
        maskT = tmp("maskT", [128, NOWN], BF16); b_maskT = Buf("maskT")
        selbp8 = tmp("selbp8", [128, 8, 128], BF16); b_selbp8 = [Buf(f"selbp8_{i}") for i in range(8)]
        gsm8 = tmp("gsm8", [128, 8, 48]); b_gsm8 = [Buf(f"gsm8_{i}") for i in range(8)]
        PT = [tmp(f"PT{i}", [128, 512], BF16) for i in range(3)]; b_PT = [Buf(f"PT{i}") for i in range(3)]
        t1 = [tmp(f"t1_{i}", [128, 512]) for i in range(2)]; b_t1 = [Buf(f"t1_{i}") for i in range(2)]
        t2 = [tmp(f"t2_{i}", [128, 512]) for i in range(2)]; b_t2 = [Buf(f"t2_{i}") for i in range(2)]
        OAt = [tmp(f"OAt{i}", [128, 128], BF16) for i in range(2)]; b_OAt = [Buf(f"OAt{i}") for i in range(2)]
        rec = tmp("rec", [128, 4]); b_rec = [Buf(f"rec{i}") for i in range(2)]
        OATs = tmp("OATs", [128, NOWN], BF16); b_OATs = Buf("OATs")
        YRTs = tmp("YRTs", [128, 2, NOWN], BF16); b_YRTs = Buf("YRTs")
        STb8 = tmp("STb8", [128, 8, 128], BF16); b_STb8 = [Buf(f"STb8_{i}") for i in range(8)]
        tcx = [tmp(f"tcx{i}", [128, 256]) for i in range(2)]; b_tcx = [Buf(f"tcx{i}") for i in range(2)]
        osb8 = tmp("osb8", [128, 8, 256]); b_osb8 = [Buf(f"osb8_{i}") for i in range(8)]
        gst8 = tmp("gst8", [128, 8, 12]); b_gst8 = [Buf(f"gst8_{i}") for i in range(8)]
        yb8 = tmp("yb8", [128, 8, 256], BF16); b_yb8 = [Buf(f"yb8_{i}") for i in range(8)]

        S.op("pool", lambda e: e.memset(V[:, :, :], 1.0), writes=b_V)
        S.op("pool", lambda e: e.memset(maskT[:, :], 0.0), writes=[b_maskT])
        S.op("pool", lambda e: e.memset(selbp8[:, :, :], 0.0), writes=b_selbp8)

        pfc = [0]; ptc = [0]; trc = [0]; rc = [0]

        def rotary_evac(bk, dst_ap, b_dst, rti, b_rti):
            r = rc[0] % 2
            rc[0] += 1
            S.op("dve", lambda e: e.tensor_tensor(out=t1[r][:, :], in0=pb[bk][:, :], in1=rti[:, 0, :], op=ALU.mult),
                 reads=[b_pb[bk], b_rti], writes=[b_t1[r]])
            S.op("dve", lambda e: e.tensor_tensor(out=t2[r][0:64, :], in0=pb[bk][64:128, :], in1=rti[0:64, 1, :], op=ALU.mult),
                 reads=[b_pb[bk], b_rti], writes=[b_t2[r]])
            S.op("dve", lambda e: e.tensor_tensor(out=t2[r][64:128, :], in0=pb[bk][0:64, :], in1=rti[64:128, 1, :], op=ALU.mult),
                 reads=[b_pb[bk], b_rti], writes=[b_t2[r]])
            S.op("pool", lambda e: e.tensor_tensor(out=dst_ap, in0=t1[r][:, :], in1=t2[r][:, :], op=ALU.add),
                 reads=[b_t1[r], b_t2[r]], writes=[b_dst])

        def proj_fm(col, hti, b_hti):
            bk = pfc[0] % 2
            pfc[0] += 1
            for c in range(16):
                S.op("pe", lambda e: e.matmul(pb[bk][:, :], lhsT=W1b[:, c, col:col + 128], rhs=hti[:, c, :],
                                              start=(c == 0), stop=(c == 15)),
                     reads=[b_W1b, b_hti], writes=[b_pb[bk]])
            return bk

        def retA(h):
            for n in range(8):
                cs = slice(n * 128, (n + 1) * 128)
                bk = stbanks[stc[0] % 3]
                stc[0] += 1
                S.op("pe", lambda e: e.matmul(pb[bk][:, 0:128], lhsT=KRT[:, cs], rhs=QRT[:, cs], start=True, stop=True),
                     reads=[b_KRT, b_QRT], writes=[b_pb[bk]])
                S.op("dve", lambda e: e.tensor_tensor(out=STb8[:, n, :], in0=pb[bk][:, 0:128], in1=dT[:, h, :], op=ALU.mult),
                     reads=[b_pb[bk], b_c1], writes=[b_STb8[n]])
            for n in range(8):
                cs = slice(n * 128, (n + 1) * 128)
                r = n % 2
                bi_, bc_ = (2, 3) if r == 0 else (7, 1)
                S.op("pe", lambda e: e.matmul(pb[bc_][:, 0:256], lhsT=QRT[:, cs], rhs=Rb[:, n, :], start=True, stop=True),
                     reads=[b_QRT, b_Rb[n]], writes=[b_pb[bc_]])
                S.op("pe", lambda e: e.matmul(pb[bi_][:, 0:256], lhsT=STb8[:, n, :], rhs=RVo[:, n, :], start=True, stop=True),
                     reads=[b_STb8[n], b_RVo[n]], writes=[b_pb[bi_]])
                S.op("act", lambda e: e.activation(out=tcx[r][:, :], in_=pb[bc_][:, 0:256], func=AF.Copy, scale=cols[:, 0, h:h + 1]),
                     reads=[b_pb[bc_], b_c1], writes=[b_tcx[r]])
                S.op("dve", lambda e: e.tensor_tensor(out=osb8[:, n, :], in0=pb[bi_][:, 0:256], in1=tcx[r][:, :], op=ALU.add),
                     reads=[b_pb[bi_], b_tcx[r]], writes=[b_osb8[n]])

        def retB(h):
            for n in range(8):
                S.op("dve", lambda e: e.bn_stats(out=gst8[:, n, 0:6], in_=osb8[:, n, :]), reads=[b_osb8[n]], writes=[b_gst8[n]])
            for n in range(8):
                S.op("dve", lambda e: e.bn_aggr(out=gst8[:, n, 6:8], in_=gst8[:, n, 0:6]), reads=[b_gst8[n]], writes=[b_gst8[n]])
            S.op("act", lambda e: e.activation(out=gst8[:, :, 8], in_=gst8[:, :, 7], func=AF.Sqrt, scale=1.0, bias=GN_EPS),
                 reads=b_gst8, writes=b_gst8)
            S.op("dve", lambda e: e.reciprocal(out=gst8[:, :, 9], in_=gst8[:, :, 8]), reads=b_gst8, writes=b_gst8)
            for n in range(8):
                S.op("dve", lambda e: e.tensor_scalar(out=osb8[:, n, :], in0=osb8[:, n, :], scalar1=gst8[:, n, 6:7],
                                                      scalar2=gst8[:, n, 9:10], op0=ALU.subtract, op1=ALU.mult),
                     reads=[b_osb8[n], b_gst8[n]], writes=[b_osb8[n]])
                S.op("pool", lambda e: e.tensor_tensor(out=yb8[:, n, :], in0=osb8[:, n, :], in1=SGo[:, n, :], op=ALU.mult),
                     reads=[b_osb8[n], b_SGo[n]], writes=[b_yb8[n]])

        def retC(h):
            for n in range(8):
                cs = slice(n * 128, (n + 1) * 128)
                tb = 4 + trc[0] % 2
                trc[0] += 1
                tpv = pbf(tb)[:, 0:256].rearrange("p (a b) -> p a b", b=128)
                for k2 in range(2):
                    S.op("pe", lambda e: e.transpose(out=tpv[:, k2, :], in_=yb8[:, n, k2 * 128:(k2 + 1) * 128], identity=ident[:, :]),
                         reads=[b_yb8[n], b_ident], writes=[b_pb[tb]])
                S.op("act", lambda e: e.copy(out=YRTs[:, :, cs], in_=tpv), reads=[b_pb[tb]], writes=[b_YRTs])
            S.dma("sp", "yr", oy_scr[1024 + h * 256:1024 + (h + 1) * 256, :].rearrange("(k p) t -> p k t", p=128),
                  YRTs[:, :, :], reads=[b_YRTs], writes=[b_oy])

        def load_grp(g):
            gi = g % 2
            S.dma("sp", f"hl{gi}", hTg[gi][:, :, :], hT_v[:, :, g * 512:(g + 1) * 512],
                  reads=[b_scr[g]], writes=[b_hTg[gi]])
            S.dma("sp", f"rl{gi}", rt[gi][:, :, :], rot_d[:, :, g * 512:(g + 1) * 512], writes=[b_rt[gi]])

        stbanks = (0, 1, 6)
        stc = [0]
        for h in range(nheads):
            cut(1)
            S.op("pool", lambda e: e.memset(Rst[:, :], 0.0), writes=[b_R])
            pendB = [None]
            for g in range(8):
                own = g >= 6
                gi = g % 2
                if not (h > 0 and g < 2):
                    load_grp(g)
                if h > 0 and g in (1, 2, 3):
                    (retA, retB, retC)[g - 1](h - 1)
                gs_ = slice(g * 512, (g + 1) * 512)
                os_ = slice((g - 6) * 512, (g - 5) * 512)
                cut(20)
                bk = proj_fm(128, hTg[gi], b_hTg[gi])
                cut(201)
                S.op("act", lambda e: e.copy(out=KT[:, gs_], in_=pb[bk][:, :]), reads=[b_pb[bk]], writes=[b_KT[g]])
                cut(202)
                S.op("dve", lambda e: e.tensor_reduce(out=kms[:, 2 * g:2 * g + 2],
                                                      in_=pb[bk][:, :].rearrange("p (a b) -> p a b", b=256),
                                                      axis=mybir.AxisListType.X, op=ALU.add),
                     reads=[b_pb[bk]], writes=[b_kms])
                cut(21)
                bk = proj_fm(384, hTg[gi], b_hTg[gi])
                kr = g % 2
                if own:
                    rotary_evac(bk, KRT[:, os_], b_KRT, rt[gi], b_rt[gi])
                    krsrc, b_krsrc = KRT, b_KRT
                    kroff = (g - 6) * 512
                else:
                    rotary_evac(bk, KRg[kr][:, :], b_KRg[kr], rt[gi], b_rt[gi])
                    krsrc, b_krsrc = KRg[kr], b_KRg[kr]
                    kroff = 0
                if own:
                    bk = proj_fm(0, hTg[gi], b_hTg[gi])
                    S.op("act", lambda e: e.copy(out=QT[:, os_], in_=pb[bk][:, :]), reads=[b_pb[bk]], writes=[b_QT])
                    bk = proj_fm(256, hTg[gi], b_hTg[gi])
                    rotary_evac(bk, QRT[:, os_], b_QRT, rt[gi], b_rt[gi])
                rvs = []
                for tl in range(4):
                    t = g * 4 + tl
                    n = t - 24
                    bk = (2, 3, 0, 1)[ptc[0] % 4]
                    ptc[0] += 1
                    for c in range(16):
                        S.op("pe", lambda e: e.matmul(pb[bk][:, 0:384], lhsT=hTg[gi][:, c, tl * 128:(tl + 1) * 128],
                                                      rhs=W1b[:, c, 512:896], start=(c == 0), stop=(c == 15)),
                             reads=[b_W1b, b_hTg[gi]], writes=[b_pb[bk]])
                    S.op("dve", lambda e: e.tensor_copy(out=V[:, t, 0:128], in_=pb[bk][:, 0:128]),
                         reads=[b_pb[bk]], writes=[b_V[g]])
                    if own:
                        rv_ap, b_rv = RVo[:, n, :], b_RVo[n]
                    else:
                        rv_ap, b_rv = RVt[t % 8][:, :], b_RVt[t % 8]
                    rvs.append((rv_ap, b_rv))
                    S.op("act", lambda e: e.copy(out=rv_ap, in_=pb[bk][:, 128:384]), reads=[b_pb[bk]], writes=[b_rv])
                    if own:
                        bk2 = (2, 3, 0, 1)[ptc[0] % 4]
                        ptc[0] += 1
                        for c in range(16):
                            S.op("pe", lambda e: e.matmul(pb[bk2][:, 0:256], lhsT=hTg[gi][:, c, tl * 128:(tl + 1) * 128],
                                                          rhs=W1b[:, c, 896:1152], start=(c == 0), stop=(c == 15)),
                                 reads=[b_W1b, b_hTg[gi]], writes=[b_pb[bk2]])
                        si = t % 2
                        S.op("act", lambda e: e.activation(out=sil[si][:, :], in_=pb[bk2][:, 0:256], func=AF.Silu),
                             reads=[b_pb[bk2]], writes=[b_sil[si]])
                        S.op("pool", lambda e: e.tensor_tensor(out=SGo[:, n, :], in0=sil[si][:, :],
                                                               in1=gnwB[:, h * 256:(h + 1) * 256], op=ALU.mult),
                             reads=[b_sil[si], b_c1], writes=[b_SGo[n]])
                def partB(g=g, own=own, kr=kr, krsrc=krsrc, b_krsrc=b_krsrc, kroff=kroff, rvs=rvs, h=h):
                    tb = 4 + trc[0] % 2
                    trc[0] += 1
                    tpv = pbf(tb)[:, 0:512].rearrange("p (a b) -> p a b", b=128)
                    for tl in range(4):
                        S.op("pe", lambda e: e.transpose(out=tpv[:, tl, :], in_=krsrc[:, kroff + tl * 128:kroff + (tl + 1) * 128],
                                                         identity=ident[:, :]),
                             reads=[b_krsrc, b_ident], writes=[b_pb[tb]])
                    S.op("act", lambda e: e.activation(out=KZ[kr][:, :, :], in_=tpv, func=AF.Copy, scale=cols[:, 1, h:h + 1]),
                         reads=[b_pb[tb], b_c1], writes=[b_KZ[kr]])
                    for tl in range(4):
                        t = g * 4 + tl
                        n = t - 24
                        rv_ap, b_rv = rvs[tl]
                        if own:
                            S.op("act", lambda e: e.copy(out=Rb[:, n, :], in_=Rst[:, :]), reads=[b_R], writes=[b_Rb[n]])
                        ub = (6, 7)[t % 2]
                        S.op("pe", lambda e: e.matmul(pb[ub][:, 0:256], lhsT=KZ[kr][:, tl, :], rhs=rv_ap, start=True, stop=True),
                             reads=[b_KZ[kr], b_rv], writes=[b_pb[ub]])
                        S.op("dve", lambda e: e.scalar_tensor_tensor(out=Rst[:, :], in0=Rst[:, :], scalar=cols[:, 2, h:h + 1],
                                                                     in1=pb[ub][:, 0:256], op0=ALU.mult, op1=ALU.add),
                             reads=[b_pb[ub], b_c1, b_R], writes=[b_R])

                if pendB[0] is not None:
                    pendB[0]()
                pendB[0] = partB
            pendB[0]()
            pendB[0] = None
            if h + 1 < nheads:
                load_grp(0)
                load_grp(1)
            cut(4)
            S.op("dve", lambda e: e.tensor_scalar(out=kmb[:, :], in0=kms[:, :], scalar1=1.0 / 256, scalar2=None, op0=ALU.mult),
                 reads=[b_kms], writes=[b_kmb])
            gbanks = (0, 1, 2, 3)
            for qt in range(8):
                i = qt // 2
                qs = slice(qt * 128, (qt + 1) * 128)
                gb = gbanks[qt % 4]
                S.op("pe", lambda e: e.matmul(pb[gb][:, 0:16], lhsT=QT[:, qs], rhs=kmb[:, :], start=True, stop=True),
                     reads=[b_QT, b_kmb], writes=[b_pb[gb]])
                S.op("dve", lambda e: e.tensor_tensor(out=gsm8[:, qt, 0:16], in0=pb[gb][:, 0:16], in1=pmask[:, i, :], op=ALU.add),
                     reads=[b_pb[gb], b_c1], writes=[b_gsm8[qt]])
            for qt in range(8):
                S.op("dve", lambda e: e.max(out=gsm8[:, qt, 16:24], in_=gsm8[:, qt, 0:16]), reads=[b_gsm8[qt]], writes=[b_gsm8[qt]])
            for qt in range(8):
                S.op("dve", lambda e: e.tensor_scalar(out=gsm8[:, qt, 24:25], in0=gsm8[:, qt, 18:19], scalar1=-1e29, scalar2=None, op0=ALU.max),
                     reads=[b_gsm8[qt]], writes=[b_gsm8[qt]])
            for qt in range(8):
                S.op("dve", lambda e: e.tensor_scalar(out=gsm8[:, qt, 32:48], in0=gsm8[:, qt, 0:16], scalar1=gsm8[:, qt, 24:25], scalar2=1.0,
                                                      op0=ALU.is_ge, op1=ALU.subtract),
                     reads=[b_gsm8[qt]], writes=[b_gsm8[qt]])
            for qt in range(8):
                S.op("dve", lambda e: e.tensor_scalar(out=selbp8[:, qt, 0:16], in0=gsm8[:, qt, 32:48], scalar1=-NEG, scalar2=None, op0=ALU.mult),
                     reads=[b_gsm8[qt]], writes=[b_selbp8[qt]])
            for qt in range(8):
                qs = slice(qt * 128, (qt + 1) * 128)
                tb = 4 + trc[0] % 2
                trc[0] += 1
                S.op("pe", lambda e: e.transpose(out=pbf(tb)[:, 0:128], in_=selbp8[:, qt, :], identity=ident[:, :]),
                     reads=[b_selbp8[qt], b_ident], writes=[b_pb[tb]])
                S.op("act", lambda e: e.copy(out=maskT[0:16, qs], in_=pbf(tb)[0:16, 0:128]), reads=[b_pb[tb]], writes=[b_maskT])

            cut(5)
            if h + 1 < nheads:
                load_w1(h + 1, eng="dve")
            stbanks = (0, 1, 6)
            stc = [0]
            obanks = (2, 3, 6, 7)
            qkc = [0]
            for P in range(2):
                items = []
                for kt in range(24):
                    items.append(dict(kt=kt, q0=512 * P, w=512, mask=("sel", kt // 2),
                                      pv=[(128 * a_, obanks[a_], False) for a_ in range(4)]))
                for qb in range(2):
                    i = 2 * P + qb
                    oa_, ob_ = obanks[2 * qb], obanks[2 * qb + 1]
                    for kt in range(24, 24 + 2 * i):
                        items.append(dict(kt=kt, q0=256 * i, w=256, mask=("sel", kt // 2), pv=[(0, oa_, False), (128, ob_, False)]))
                    items.append(dict(kt=24 + 2 * i, q0=256 * i, w=256, mask=("dm",), pv=[(0, oa_, True), (128, ob_, False)]))
                    items.append(dict(kt=24 + 2 * i + 1, q0=256 * i + 128, w=128, mask=("dm",), pv=[(0, ob_, True)]))
                first = {ob: True for ob in obanks}

                def emit_qk(it):
                    bk = (0, 1)[qkc[0] % 2]
                    pi = qkc[0] % 3
                    qkc[0] += 1
                    kt, q0, w = it["kt"], it["q0"], it["w"]
                    kts = slice(kt * 128, (kt + 1) * 128)
                    g_ = kt // 4
                    S.op("pe", lambda e: e.matmul(pb[bk][:, 0:w], lhsT=KT[:, kts], rhs=QT[:, q0:q0 + w], start=True, stop=False),
                         reads=[b_KT[g_], b_QT], writes=[b_pb[bk]])
                    if it["mask"][0] == "sel":
                        j = it["mask"][1]
                        S.op("pe", lambda e: e.matmul(pb[bk][:, 0:w], lhsT=selhot[:, j, :], rhs=maskT[:, q0:q0 + w], start=False, stop=True),
                             reads=[b_c1, b_maskT], writes=[b_pb[bk]])
                    else:
                        S.op("pe", lambda e: e.matmul(pb[bk][:, 0:w], lhsT=ident[:, :], rhs=dmask[:, 0:w], start=False, stop=True),
                             reads=[b_ident, b_c1], writes=[b_pb[bk]])
                    S.op("act", lambda e: e.activation(out=PT[pi][:, 0:w], in_=pb[bk][:, 0:w], func=AF.Exp, scale=SCALE),
                         reads=[b_pb[bk]], writes=[b_PT[pi]])
                    it["pi"] = pi

                def emit_pv(it):
                    kt, pi = it["kt"], it["pi"]
                    g_ = kt // 4
                    for pc, ob, last in it["pv"]:
                        S.op("pe", lambda e: e.matmul(pb[ob][:, 0:129], lhsT=PT[pi][:, pc:pc + 128], rhs=V[:, kt, :],
                                                      start=first[ob], stop=last),
                             reads=[b_PT[pi], b_V[g_]], writes=[b_pb[ob]])
                        first[ob] = False

                pend = []
                for it in items:
                    emit_qk(it)
                    pend.append(it)
                    if len(pend) > 2:
                        emit_pv(pend.pop(0))
                while pend:
                    emit_pv(pend.pop(0))
                for a_ in range(4):
                    qt = 4 * P + a_
                    ob = obanks[a_]
                    ri = qt % 2
                    S.op("dve", lambda e: e.reciprocal(out=rec[:, ri:ri + 1], in_=pb[ob][:, 128:129]),
                         reads=[b_pb[ob]], writes=[b_rec[ri]])
                    S.op("dve", lambda e: e.tensor_scalar(out=OAt[ri][:, :], in0=pb[ob][:, 0:128], scalar1=rec[:, ri:ri + 1],
                                                          scalar2=None, op0=ALU.mult),
                         reads=[b_pb[ob], b_rec[ri]], writes=[b_OAt[ri]])
                    tb = 4 + trc[0] % 2
                    trc[0] += 1
                    S.op("pe", lambda e: e.transpose(out=pbf(tb)[:, 0:128], in_=OAt[ri][:, :], identity=ident[:, :]),
                         reads=[b_OAt[ri], b_ident], writes=[b_pb[tb]])
                    S.op("act", lambda e: e.copy(out=OATs[:, qt * 128:(qt + 1) * 128], in_=pbf(tb)[:, 0:128]),
                         reads=[b_pb[tb]], writes=[b_OATs])
            S.dma("sp", "oa", oy_scr[h * 128:(h + 1) * 128, :], OATs[:, :], reads=[b_OATs], writes=[b_oy])

            cut(6)
        retA(nheads - 1)
        retB(nheads - 1)
        retC(nheads - 1)
        S.barrier()
        if upto == "p1" and debug:
            for nm, t_ in [("KT", KT), ("QT", QT), ("QRT", QRT), ("KRT", KRT), ("V", V), ("kms", kms), ("maskT", maskT),
                           ("Rst", Rst), ("Rb", Rb), ("RVo", RVo), ("SGo", SGo), ("OATs", OATs), ("YRTs", YRTs), ("W1b", W1b)]:
                shp = list(t_.shape)
                d_ = dbg_out("dbg_" + nm, shp, t_.dtype)
                sl = tuple(slice(None) for _ in shp)
                S.dma("sp", "dd_" + nm, d_[sl], t_[sl])
            S.barrier()

    except _Cut:
        return nc, dbg
    es01.close()

    if upto == "p1":
        d = dbg_out("dbg_oy", [3072, NOWN], BF16)
        with ExitStack() as es:
            t_ = es.enter_context(nc.sbuf_tensor("dbgt", [128, 24, NOWN], BF16)); b_t = Buf("dbgt")
            for kk in range(0, 24, 8):
                S.dma("sp", f"dl{kk}", t_[:, kk:kk + 8, :], oy_scr.rearrange("(k p) t -> p k t", p=128)[:, kk:kk + 8, :], reads=[b_oy], writes=[b_t])
                S.dma("sp", f"ds{kk}", d.rearrange("(k p) t -> p k t", p=128)[:, kk:kk + 8, :], t_[:, kk:kk + 8, :], reads=[b_t])
            S.barrier()
        return nc, dbg

    bufA = nc.alloc_sbuf_tensor("s_bufA", [128, 16, NOWN], BF16)
    b_xres = [Buf(f"xres{t}") for t in range(8)]
    sig_f = AF.Sigmoid
    if True:
        mixT = bufA; b_mixT = [Buf(f"mixT{i}") for i in range(2)]
        with ExitStack() as es:
            def tmp(name, shape, dt=F32):
                UNIQ[0] += 1
                return es.enter_context(nc.sbuf_tensor(f"t{UNIQ[0]}_" + name, list(shape), dt))
            hTo = tmp("hTo", [128, 16, NOWN], BF16); b_hTo = Buf("hTo")
            OAT = tmp("OAT", [128, 8, NOWN], BF16); YRT = tmp("YRT", [128, 16, NOWN], BF16); b_OY = Buf("OY")
            for g in (6, 7):
                S.dma("sp", f"ho{g}", hTo[:, :, (g - 6) * 512:(g - 5) * 512], hT_v[:, :, g * 512:(g + 1) * 512],
                      reads=[b_scr[g]], writes=[b_hTo])
            S.dma("sp", "oal", OAT[:, :, :], oy_scr[0:1024, :].rearrange("(k p) t -> p k t", p=128), reads=[b_oy], writes=[b_OY])
            for k8 in range(2):
                S.dma("sp", f"yrl{k8}", YRT[:, 8 * k8:8 * k8 + 8, :],
                      oy_scr[1024 + 1024 * k8:2048 + 1024 * k8, :].rearrange("(k p) t -> p k t", p=128), reads=[b_oy], writes=[b_OY])
            stgX = [tmp(f"stgX{i}", [128, 16, 128]) for i in range(4)]; b_stgX = [Buf(f"stgX{i}") for i in range(4)]
            sx = [0]
            Wgb = [tmp(f"Wgb{i}", [128, 16, 256], BF16) for i in range(2)]; b_Wgb = [Buf(f"Wgb{i}") for i in range(2)]
            Wmb = [tmp(f"Wmb{i}", [128, 8, 128], BF16) for i in range(2)]; b_Wmb = [Buf(f"Wmb{i}") for i in range(2)]
            Wrb = [tmp(f"Wrb{i}", [128, 16, 128], BF16) for i in range(2)]; b_Wrb = [Buf(f"Wrb{i}") for i in range(2)]
            sg = [tmp(f"sg{i}", [128, 512]) for i in range(4)]; b_sg = [Buf(f"sg{i}") for i in range(4)]
            tt = [tmp(f"tt{i}", [128, 512]) for i in range(4)]; b_tt = [Buf(f"tt{i}") for i in range(4)]

            def load_fc(fc):
                k = fc % 2

                def one(src_ap, nchunk, dst_ap, b_dst, fold):
                    i_ = sx[0] % 4
                    sx[0] += 1
                    S.dma("act", f"sx{i_}", stgX[i_][:, 0:nchunk, :], src_ap, writes=[b_stgX[i_]])
                    if fold:
                        S.op("pool", lambda e: e.tensor_tensor(out=dst_ap, in0=stgX[i_][:, 0:nchunk, :], in1=bc_last(nw1T, 16, 128), op=ALU.mult),
                             reads=[b_stgX[i_], b_nw], writes=[b_dst])
                    else:
                        S.op("pool", lambda e: e.tensor_copy(out=dst_ap, in_=stgX[i_][:, 0:nchunk, :]), reads=[b_stgX[i_]], writes=[b_dst])
                one(w_in[:, 9216 + fc * 128:9216 + (fc + 1) * 128].rearrange("(c p) n -> p c n", p=128), 16, Wgb[k][:, :, 0:128], b_Wgb[k], True)
                one(w_in[:, 11264 + fc * 128:11264 + (fc + 1) * 128].rearrange("(c p) n -> p c n", p=128), 16, Wgb[k][:, :, 128:256], b_Wgb[k], True)
                one(wbm[:, fc * 128:(fc + 1) * 128].rearrange("(c p) n -> p c n", p=128), 8, Wmb[k][:, :, :], b_Wmb[k], False)
                one(wbr[:, fc * 128:(fc + 1) * 128].rearrange("(c p) n -> p c n", p=128), 16, Wrb[k][:, :, :], b_Wrb[k], False)

            load_fc(0)
            for fc in range(16):
                k = fc % 2
                if fc + 1 < 16:
                    load_fc(fc + 1)
                for th in range(2):
                    ts_ = slice(th * 512, (th + 1) * 512)
                    st_ = (2 * fc + th) % 2
                    base = 4 * st_
                    for c in range(16):
                        S.op("pe", lambda e: e.matmul(pb[base][:, :], lhsT=Wgb[k][:, c, 0:128], rhs=hTo[:, c, ts_], start=(c == 0), stop=(c == 15)),
                             reads=[b_Wgb[k], b_hTo], writes=[b_pb[base]])
                    for c in range(16):
                        S.op("pe", lambda e: e.matmul(pb[base + 1][:, :], lhsT=Wgb[k][:, c, 128:256], rhs=hTo[:, c, ts_], start=(c == 0), stop=(c == 15)),
                             reads=[b_Wgb[k], b_hTo], writes=[b_pb[base + 1]])
                    for c in range(8):
                        S.op("pe", lambda e: e.matmul(pb[base + 2][:, :], lhsT=Wmb[k][:, c, :], rhs=OAT[:, c, ts_], start=(c == 0), stop=(c == 7)),
                             reads=[b_Wmb[k], b_OY], writes=[b_pb[base + 2]])
                    for c in range(16):
                        S.op("pe", lambda e: e.matmul(pb[base + 3][:, :], lhsT=Wrb[k][:, c, :], rhs=YRT[:, c, ts_], start=(c == 0), stop=(c == 15)),
                             reads=[b_Wrb[k], b_OY], writes=[b_pb[base + 3]])
                    i0, i1 = 2 * st_, 2 * st_ + 1
                    S.op("act", lambda e: e.activation(out=sg[i0][:, :], in_=pb[base][:, :], func=sig_f), reads=[b_pb[base]], writes=[b_sg[i0]])
                    S.op("act", lambda e: e.activation(out=sg[i1][:, :], in_=pb[base + 1][:, :], func=sig_f), reads=[b_pb[base + 1]], writes=[b_sg[i1]])
                    S.op("dve", lambda e: e.tensor_tensor(out=tt[i0][:, :], in0=pb[base + 2][:, :], in1=sg[i0][:, :], op=ALU.mult),
                         reads=[b_pb[base + 2], b_sg[i0]], writes=[b_tt[i0]])
                    S.op("dve", lambda e: e.tensor_tensor(out=tt[i1][:, :], in0=pb[base + 3][:, :], in1=sg[i1][:, :], op=ALU.mult),
                         reads=[b_pb[base + 3], b_sg[i1]], writes=[b_tt[i1]])
                    S.op("pool", lambda e: e.tensor_tensor(out=mixT[:, fc, ts_], in0=tt[i0][:, :], in1=tt[i1][:, :], op=ALU.add),
                         reads=[b_tt[i0], b_tt[i1]], writes=[b_mixT[th]])
            S.barrier()

        if upto == "p2m":
            d = dbg_out("dbg_mixT", [128, 16, NOWN], BF16)
            S.dma("sp", "dmix", d[:, :, :], mixT[:, :, :], reads=b_mixT)
            S.barrier()
            return nc, dbg

        xres = nc.alloc_sbuf_tensor("s_xres", [128, 8, D], F32)
        with ExitStack() as es:
            def tmp(name, shape, dt=F32):
                UNIQ[0] += 1
                return es.enter_context(nc.sbuf_tensor(f"t{UNIQ[0]}_" + name, list(shape), dt))
            for t in range(8):
                S.dma("sp", f"xr{t}", xres[:, t, :], xs[OWN0 + t * 128:OWN0 + (t + 1) * 128, :], writes=[b_xres[t]])
            stgW = [tmp(f"stgW{i}", [128, 8, 512]) for i in range(2)]; b_stgW = [Buf(f"stgW{i}") for i in range(2)]
            Wob = [tmp(f"Wob{i}", [128, 16, 512], BF16) for i in range(2)]; b_Wob = [[Buf(f"Wob{i}_{hf}") for hf in range(2)] for i in range(2)]
            sc = [0]

            def load_wo(cg):
                k = cg % 2
                v = wout[:, cg * 512:(cg + 1) * 512].rearrange("(c p) n -> p c n", p=128)
                for half in range(2):
                    s_ = sc[0] % 2
                    sc[0] += 1
                    S.dma("act", f"wo{s_}", stgW[s_][:, :, :], v[:, 8 * half:8 * half + 8, :], writes=[b_stgW[s_]])
                    S.op("pool", lambda e: e.tensor_copy(out=Wob[k][:, 8 * half:8 * half + 8, :], in_=stgW[s_][:, :, :]),
                         reads=[b_stgW[s_]], writes=[b_Wob[k][half]])
            load_wo(0)
            bc_ = [0]
            for cg in range(4):
                k = cg % 2
                if cg + 1 < 4:
                    load_wo(cg + 1)
                for t in range(8):
                    bk = bc_[0] % 8
                    bc_[0] += 1
                    for c in range(16):
                        S.op("pe", lambda e: e.matmul(pb[bk][:, :], lhsT=mixT[:, c, t * 128:(t + 1) * 128], rhs=Wob[k][:, c, :],
                                                      start=(c == 0), stop=(c == 15)),
                             reads=[b_Wob[k][c // 8]] + b_mixT, writes=[b_pb[bk]])
                    S.op("dve", lambda e: e.tensor_tensor(out=xres[:, t, cg * 512:(cg + 1) * 512], in0=pb[bk][:, :],
                                                          in1=xres[:, t, cg * 512:(cg + 1) * 512], op=ALU.add),
                         reads=[b_pb[bk], b_xres[t]], writes=[b_xres[t]])
            S.barrier()

    if upto == "p2a":
        d = dbg_out("dbg_x2", [NOWN, D])
        for t in range(8):
            S.dma("sp", f"dx{t}", d[t * 128:(t + 1) * 128, :], xres[:, t, :], reads=[b_xres[t]])
        S.barrier()
        return nc, dbg

    with ExitStack() as es:
        def tmp(name, shape, dt=F32):
            UNIQ[0] += 1
            return es.enter_context(nc.sbuf_tensor(f"t{UNIQ[0]}_" + name, list(shape), dt))
        h2T = bufA; b_h2T = Buf("h2T"); b_h2Tp = [Buf(f"h2Tp{i}") for i in range(32)]
        Wring = [tmp(f"Wring{i}", [128, 8192], BF16) for i in range(3)]; b_Wring = [Buf(f"Wring{i}") for i in range(3)]
        stgE = [tmp(f"stgE{i}", [128, 2048]) for i in range(3)]; b_stgE = [Buf(f"stgE{i}") for i in range(3)]
        actT = tmp("actT", [128, 4, NOWN], BF16); b_actT = [Buf(f"actT{i}") for i in range(2)]
        silt = [tmp(f"silt{i}", [128, 512]) for i in range(2)]; b_silt = [Buf(f"silt{i}") for i in range(2)]
        comb = tmp("comb", [128, 8, 32]); b_combs = [Buf(f"comb{t}") for t in range(8)]
        rs = tmp("rs", [128, 64]); b_rs = [Buf(f"rs{i}") for i in range(2)]
        wr36s = tmp("wr36s", [128, 16, 36]); wr36b = tmp("wr36b", [128, 16, 36], BF16); b_wr36 = Buf("wr36")
        brB = tmp("brB", [128, 36]); b_brB = Buf("brB")
        S.dma("sp", "wr36", wr36s[:, :, :], wr36_d.rearrange("(c p) n -> p c n", p=128), writes=[b_wr36])
        S.dma("sp", "brB", brB[:, :], bass.AP(br36_d, 0, [[0, 128], [1, 36]]), writes=[b_brB])
        S.op("pool", lambda e: e.tensor_tensor(out=wr36b[:, :, :], in0=wr36s[:, :, :], in1=bc_last(nw2T, 16, 36), op=ALU.mult),
             reads=[b_wr36, b_nw], writes=[b_wr36])

        wc = [0]; sce = [0]

        def load_mat(kind, e_):
            k = wc[0] % 3
            wc[0] += 1
            if kind in ("g", "u"):
                v = (weg if kind == "g" else weu)[e_].rearrange("(c p) n -> p c n", p=128)
                wv = Wring[k][:, :].rearrange("p (c n) -> p c n", n=512)
                for q4 in range(4):
                    s_ = sce[0] % 3
                    sce[0] += 1
                    sv = stgE[s_][:, :].rearrange("p (c n) -> p c n", n=512)
                    S.dma("sp", f"se{s_}", sv, v[:, 4 * q4:4 * q4 + 4, :], writes=[b_stgE[s_]])
                    if kind == "g":
                        S.op("pool", lambda e: e.tensor_tensor(out=wv[:, 4 * q4:4 * q4 + 4, :], in0=sv,
                                                               in1=bc_last(nw2T, 4, 512, off=4 * q4), op=ALU.mult),
                             reads=[b_stgE[s_], b_nw], writes=[b_Wring[k]])
                    else:
                        for c in range(4):
                            cc = 4 * q4 + c
                            S.op("act", lambda e: e.activation(out=wv[:, cc, :], in_=sv[:, c, :], func=AF.Copy, scale=nw2T[:, cc:cc + 1]),
                                 reads=[b_stgE[s_], b_nw], writes=[b_Wring[k]])
            else:
                v = wed[e_].rearrange("(f p) n -> p f n", p=128)
                wv = Wring[k][:, :].rearrange("p (f n) -> p f n", n=2048)
                for q4 in range(4):
                    s_ = sce[0] % 3
                    sce[0] += 1
                    S.dma("sp", f"se{s_}", stgE[s_][:, :], v[:, q4, :], writes=[b_stgE[s_]])
                    S.op("dve", lambda e: e.tensor_copy(out=wv[:, q4, :], in_=stgE[s_][:, :]),
                         reads=[b_stgE[s_]], writes=[b_Wring[k]])
            return k

        kg = load_mat("g", 0)
        st = tmp("st2", [128, 8]); b_st = [Buf(f"st2_{i}") for i in range(4)]
        with ExitStack() as es3:
            def tmp3(name, shape, dt=F32):
                UNIQ[0] += 1
                return es3.enter_context(nc.sbuf_tensor(f"t{UNIQ[0]}_" + name, list(shape), dt))
            junk = tmp3("junk2", [128, D], BF16); b_junk = Buf("junk2")
            hb = [tmp3(f"hb2_{i}", [128, D], BF16) for i in range(2)]; b_hb = [Buf(f"hb2_{i}") for i in range(2)]

            def n2_a(t):
                j = t % 4
                hk = t % 2
                rms_tile(xres[:, t, :], b_xres[t], junk, b_junk, st, b_st[j], j)
                S.op("dve", lambda e: e.tensor_scalar(out=hb[hk][:, :], in0=xres[:, t, :], scalar1=st[:, 2 * j + 1:2 * j + 2],
                                                      scalar2=None, op0=ALU.mult),
                     reads=[b_xres[t], b_st[j]], writes=[b_hb[hk]])

            def n2_b(t):
                hk = t % 2
                transpose_tile(hb[hk], b_hb[hk], lambda cq: h2T[:, 4 * cq:4 * cq + 4, t * 128:(t + 1) * 128],
                               lambda cq: b_h2Tp[t * 4 + cq])
            n2_a(0)
            for t in range(8):
                if t + 1 < 8:
                    n2_a(t + 1)
                n2_b(t)
            ku = load_mat("u", 0)
            S.barrier()

        Ls = tmp("Ls", [128, 8, 36]); b_L = [Buf(f"L{t}") for t in range(8)]
        rs8 = tmp("rs8", [128, 8, 32]); b_rs8 = [Buf(f"rs8_{t}") for t in range(8)]
        rbanks = (6, 7, 4, 5)
        T8 = range(8)
        for t in T8:
            rb_ = rbanks[t % 4]
            for c in range(16):
                S.op("pe", lambda e: e.matmul(pb[rb_][:, 0:36], lhsT=h2T[:, c, t * 128:(t + 1) * 128], rhs=wr36b[:, c, :],
                                              start=(c == 0), stop=(c == 15)),
                     reads=[b_h2T, b_wr36], writes=[b_pb[rb_]])
            S.op("dve", lambda e: e.tensor_tensor(out=Ls[:, t, :], in0=pb[rb_][:, 0:36], in1=brB[:, :], op=ALU.add),
                 reads=[b_pb[rb_], b_brB], writes=[b_L[t]])

        def rstage(fn, rd_L=True, wr_L=False, eng="dve"):
            for t in T8:
                rds = [b_rs8[t]] + ([b_L[t]] if rd_L else [])
                wrs = [b_L[t]] if wr_L else [b_rs8[t]]
                S.op(eng, (lambda t_: (lambda e: fn(e, t_)))(t), reads=rds, writes=wrs)
        rstage(lambda e, t: e.tensor_reduce(out=rs8[:, t, 0:1], in_=Ls[:, t, 0:4], axis=mybir.AxisListType.X, op=ALU.max))
        rstage(lambda e, t: e.tensor_scalar(out=rs8[:, t, 1:2], in0=rs8[:, t, 0:1], scalar1=-1.0, scalar2=None, op0=ALU.mult))
        rstage(lambda e, t: e.tensor_scalar(out=rs8[:, t, 4:8], in0=Ls[:, t, 0:4], scalar1=rs8[:, t, 0:1], scalar2=None, op0=ALU.is_equal))
        rstage(lambda e, t: e.activation(out=rs8[:, t, 8:12], in_=Ls[:, t, 0:4], func=AF.Exp, bias=rs8[:, t, 1:2],
                                         accum_out=rs8[:, t, 2:3]), eng="act")
        rstage(lambda e, t: e.reciprocal(out=rs8[:, t, 3:4], in_=rs8[:, t, 2:3]))
        rstage(lambda e, t: e.tensor_scalar(out=rs8[:, t, 12:20], in0=Ls[:, t, 4:12], scalar1=rs8[:, t, 4:5], scalar2=None, op0=ALU.mult))
        for g in range(1, 4):
            rstage(lambda e, t, g=g: e.scalar_tensor_tensor(out=rs8[:, t, 12:20], in0=Ls[:, t, 4 + 8 * g:12 + 8 * g],
                                                            scalar=rs8[:, t, 4 + g:5 + g], in1=rs8[:, t, 12:20],
                                                            op0=ALU.mult, op1=ALU.add))
        rstage(lambda e, t: e.max(out=rs8[:, t, 20:28], in_=rs8[:, t, 12:20]))
        rstage(lambda e, t: e.tensor_tensor(out=rs8[:, t, 28:29], in0=rs8[:, t, 20:21], in1=rs8[:, t, 21:22], op=ALU.subtract))
        rstage(lambda e, t: e.activation(out=rs8[:, t, 29:30], in_=rs8[:, t, 28:29], func=AF.Exp, scale=-1.0), eng="act")
        rstage(lambda e, t: e.tensor_scalar(out=rs8[:, t, 29:30], in0=rs8[:, t, 29:30], scalar1=1.0, scalar2=None, op0=ALU.add))
        rstage(lambda e, t: e.reciprocal(out=rs8[:, t, 29:30], in_=rs8[:, t, 29:30]))
        rstage(lambda e, t: e.tensor_scalar(out=rs8[:, t, 30:31], in0=rs8[:, t, 29:30], scalar1=-1.0, scalar2=1.0,
                                            op0=ALU.mult, op1=ALU.add))
        rstage(lambda e, t: e.tensor_scalar(out=Ls[:, t, 4:12], in0=rs8[:, t, 12:20], scalar1=rs8[:, t, 20:21],
                                            scalar2=rs8[:, t, 29:30], op0=ALU.is_equal, op1=ALU.mult), wr_L=True)
        rstage(lambda e, t: e.tensor_scalar(out=Ls[:, t, 12:20], in0=rs8[:, t, 12:20], scalar1=rs8[:, t, 21:22],
                                            scalar2=rs8[:, t, 30:31], op0=ALU.is_equal, op1=ALU.mult), wr_L=True)
        rstage(lambda e, t: e.tensor_tensor(out=Ls[:, t, 4:12], in0=Ls[:, t, 4:12], in1=Ls[:, t, 12:20], op=ALU.add), wr_L=True)
        rstage(lambda e, t: e.tensor_scalar(out=rs8[:, t, 8:12], in0=rs8[:, t, 4:8], scalar1=rs8[:, t, 3:4], scalar2=None, op0=ALU.mult))
        for g in range(4):
            for t in T8:
                S.op("dve", lambda e: e.tensor_scalar(out=comb[:, t, 8 * g:8 * g + 8], in0=Ls[:, t, 4:12], scalar1=rs8[:, t, 8 + g:9 + g],
                                                      scalar2=None, op0=ALU.mult),
                     reads=[b_L[t], b_rs8[t]], writes=[b_combs[t]])

        if upto == "p2r":
            d = dbg_out("dbg_comb", [128, 8, 32])
            S.dma("sp", "dcomb", d[:, :, :], comb[:, :, :], reads=b_combs)
            S.barrier()
            return nc, dbg

        kd = load_mat("d", 0)
        gc = [0]; yc = [0]
        for e_ in range(NEXP):
            wg_v = Wring[kg][:, :].rearrange("p (c n) -> p c n", n=512)
            wu_v = Wring[ku][:, :].rearrange("p (c n) -> p c n", n=512)
            for th in range(2):
                ts_ = slice(th * 512, (th + 1) * 512)
                for fq in range(4):
                    pr = gc[0] % 2
                    gc[0] += 1
                    bg, bu = 2 * pr, 2 * pr + 1
                    for c in range(16):
                        S.op("pe", lambda e: e.matmul(pb[bg][:, :], lhsT=wg_v[:, c, fq * 128:(fq + 1) * 128], rhs=h2T[:, c, ts_],
                                                      start=(c == 0), stop=(c == 15)),
                             reads=[b_Wring[kg], b_h2T], writes=[b_pb[bg]])
                    for c in range(16):
                        S.op("pe", lambda e: e.matmul(pb[bu][:, :], lhsT=wu_v[:, c, fq * 128:(fq + 1) * 128], rhs=h2T[:, c, ts_],
                                                      start=(c == 0), stop=(c == 15)),
                             reads=[b_Wring[ku], b_h2T], writes=[b_pb[bu]])
                    S.op("act", lambda e: e.activation(out=silt[pr][:, :], in_=pb[bg][:, :], func=AF.Silu),
                         reads=[b_pb[bg]], writes=[b_silt[pr]])
                    S.op("dve", lambda e: e.tensor_tensor(out=actT[:, fq, ts_], in0=pb[bu][:, :], in1=silt[pr][:, :], op=ALU.mult),
                         reads=[b_pb[bu], b_silt[pr]], writes=[b_actT[th]])
            if e_ + 1 < NEXP:
                kg_n = load_mat("g", e_ + 1)
                ku_n = load_mat("u", e_ + 1)
            wd_v = Wring[kd][:, :].rearrange("p (f n) -> p f n", n=2048)
            for t in range(8):
                for cg in range(4):
                    bk = 4 + yc[0] % 4
                    yc[0] += 1
                    for fq in range(4):
                        S.op("pe", lambda e: e.matmul(pb[bk][:, :], lhsT=actT[:, fq, t * 128:(t + 1) * 128],
                                                      rhs=wd_v[:, fq, cg * 512:(cg + 1) * 512], start=(fq == 0), stop=(fq == 3)),
                             reads=[b_Wring[kd], b_actT[t // 4]], writes=[b_pb[bk]])
                    S.op("dve", lambda e: e.scalar_tensor_tensor(out=xres[:, t, cg * 512:(cg + 1) * 512], in0=pb[bk][:, :],
                                                                 scalar=comb[:, t, e_:e_ + 1], in1=xres[:, t, cg * 512:(cg + 1) * 512],
                                                                 op0=ALU.mult, op1=ALU.add),
                         reads=[b_pb[bk], b_combs[t], b_xres[t]], writes=[b_xres[t]])
            if e_ + 1 < NEXP:
                kd = load_mat("d", e_ + 1)
                kg, ku = kg_n, ku_n
        S.barrier()

    with ExitStack() as es:
        def tmp(name, shape, dt=F32):
            UNIQ[0] += 1
            return es.enter_context(nc.sbuf_tensor(f"t{UNIQ[0]}_" + name, list(shape), dt))
        junk = tmp("junk3", [128, D], BF16); b_junk = Buf("junk3")
        nwfB = tmp("nwfB", [128, D]); b_nwf = Buf("nwf")
        S.dma("sp", "nwf", nwfB[:, :], bass.AP(nwf_d, 0, [[0, 128], [1, D]]), writes=[b_nwf])
        ot = [tmp(f"ot{i}", [128, D]) for i in range(2)]; b_ot = [Buf(f"ot{i}") for i in range(2)]
        st = tmp("st3", [128, 8]); b_st = [Buf(f"st3_{i}") for i in range(4)]
        toks = []
        for t in range(8):
            j = t % 4
            rms_tile(xres[:, t, :], b_xres[t], junk, b_junk, st, b_st[j], j)
            k = t % 2
            S.op("dve", lambda e: e.scalar_tensor_tensor(out=ot[k][:, :], in0=xres[:, t, :], scalar=st[:, 2 * j + 1:2 * j + 2],
                                                         in1=nwfB[:, :], op0=ALU.mult, op1=ALU.mult),
                 reads=[b_xres[t], b_st[j], b_nwf], writes=[b_ot[k]])
            toks.append(S.dma("sp", f"out{k}", out[t * 128:(t + 1) * 128, :], ot[k][:, :], reads=[b_ot[k]]))
        S.barrier()
    return nc, dbg


def host_consts(qi):
    f32 = np.float32
    bf = ml_dtypes.bfloat16
    c = {}
    c["ident"] = np.eye(128, dtype=f32).astype(bf)
    kk = np.arange(128)[:, None]; qq = np.arange(128)[None, :]
    causal = np.where(kk <= qq, 0.0, NEG).astype(f32)
    c["dmask"] = np.concatenate([causal, np.zeros((128, 128), f32)], axis=1).astype(bf)
    sh = np.zeros((128, 16, 128), f32)
    for j in range(16):
        sh[j, j, :] = 1.0
    c["selhot"] = sh.astype(bf)
    pm = np.full((4, 16), -1e30, f32)
    for i in range(4):
        pm[i, 4 * (3 - qi):12 + i] = 0.0
    c["pmask"] = np.ascontiguousarray(np.broadcast_to(pm[None], (128, 4, 16)))
    half = 64
    inv = (np.float32(10000.0) ** (-(np.arange(half, dtype=f32) / np.float32(half)))).astype(f32)
    pos = (np.arange(NSLOT) - (3 - qi) * 1024).astype(f32)
    ang = (pos[:, None] * inv[None, :]).astype(f32)
    cs, sn = np.cos(ang).astype(f32).T, np.sin(ang).astype(f32).T
    rot = np.zeros((128, 2, NSLOT), f32)
    rot[0:64, 0] = cs; rot[64:128, 0] = cs
    rot[0:64, 1] = -sn; rot[64:128, 1] = sn
    c["rot"] = rot
    hh = np.arange(NH, dtype=f32)
    lg = np.log(f32(1.0) - f32(2.0) ** (f32(-5.0) - hh)).astype(f32)
    idx = np.arange(128, dtype=f32)
    diff = idx[None, :] - idx[:, None]
    dk = f32(128.0 ** -0.5)
    dT = np.zeros((128, NH, 128), f32)
    for h in range(NH):
        dT[:, h, :] = np.where(diff >= 0, np.exp(lg[h] * np.maximum(diff, 0.0)), 0.0) * dk
    c["dT"] = dT
    cols = np.zeros((128, 3, NH), f32)
    for h in range(NH):
        cols[:, 0, h] = np.exp(lg[h] * (idx + 1.0))
        cols[:, 1, h] = np.exp(lg[h] * (127.0 - idx)) * dk
        cols[:, 2, h] = np.exp(lg[h] * 128.0)
    c["cols"] = cols
    return c


def make_in_maps(x, norm_mix_w, w_in, ret_gn_w, w_branch_moba, w_branch_ret, w_out, norm_ffn_w,
                 w_router_group, b_router_group, w_router_expert, b_router_expert,
                 w_expert_gate, w_expert_up, w_expert_down, norm_final_w):
    f32 = np.float32
    x = np.asarray(x, f32)
    shared = {
        "w_in": np.ascontiguousarray(np.asarray(w_in, f32)[0]),
        "nw1T": np.ascontiguousarray(np.asarray(norm_mix_w, f32)[0].reshape(16, 128).T),
        "nw2T": np.ascontiguousarray(np.asarray(norm_ffn_w, f32)[0].reshape(16, 128).T),
        "nwf": np.asarray(norm_final_w, f32).reshape(1, D),
        "gnw": np.asarray(ret_gn_w, f32)[0].reshape(1, D),
        "wbm": np.ascontiguousarray(np.asarray(w_branch_moba, f32)[0]),
        "wbr": np.ascontiguousarray(np.asarray(w_branch_ret, f32)[0]),
        "wout": np.ascontiguousarray(np.asarray(w_out, f32)[0]),
        "wr36": np.ascontiguousarray(np.concatenate(
            [np.asarray(w_router_group, f32)[0]] + [np.asarray(w_router_expert, f32)[0, g] for g in range(4)], axis=1)),
        "br36": np.concatenate([np.asarray(b_router_group, f32)[0].reshape(-1),
                                np.asarray(b_router_expert, f32)[0].reshape(-1)]).reshape(1, 36),
        "weg": np.ascontiguousarray(np.asarray(w_expert_gate, f32)[0]),
        "weu": np.ascontiguousarray(np.asarray(w_expert_up, f32)[0]),
        "wed": np.ascontiguousarray(np.asarray(w_expert_down, f32)[0]),
    }
    consts = [host_consts(qi) for qi in range(4)]
    in_maps = []
    for c in range(8):
        b, qi = divmod(c, 4)
        xsl = np.zeros((NSLOT, D), f32)
        n = (qi + 1) * 1024
        xsl[NSLOT - n:] = x[b, :n]
        m = dict(shared)
        m.update(consts[qi])
        m["xs"] = xsl
        in_maps.append(m)
    return in_maps


_NC_CACHE = {}


def kernel(**inputs):
    in_maps = make_in_maps(**inputs)
    if "full" not in _NC_CACHE:
        _NC_CACHE["full"] = build("full")[0]
    nc = _NC_CACHE["full"]
    res = run_bass_kernel_spmd(nc, in_maps, core_ids=list(range(8)))
    outp = np.zeros((2, 4096, D), np.float32)
    for c in range(8):
        b, qi = divmod(c, 4)
        outp[b, qi * 1024:(qi + 1) * 1024] = res.results[c]["out"]
    return outp
```
